# Optimizing a Trainium2 kernel written in Bass

```python
import jax
import jax.numpy as jnp
from jax import lax
import numpy as np

D_MODEL = 2048
BATCH = 4
SEQ = 2048
DEPTH = 4

N_MIXERS = 2
ROPE_THETA = 500000.0
LN_EPS = 1e-5
RMS_EPS = 1e-6
DEEPNORM_ALPHA = (2.0 * DEPTH) ** 0.25
DEEPNORM_BETA = (8.0 * DEPTH) ** -0.25
ADA_SCALE = 0.1

MLA_HEADS = D_MODEL // 128
MLA_Q_LORA = D_MODEL // 4
MLA_KV_LORA = D_MODEL // 4
MLA_NOPE = 128
MLA_ROPE = 64
MLA_V = 128
ATTN_Q_BLOCK = 128

MOBA_HEADS = D_MODEL // 128
MOBA_HEAD_DIM = 128
MOBA_ROT_DIM = MOBA_HEAD_DIM // 4
MOBA_BLOCK = 256
MOBA_TOPK = 3
MOBA_Q_CHUNK = 16

D_FF = 5632
N_EXPERTS = 8
TOP_K = 2
MOE_ROW_BLOCK = 128

N_EVEN = (DEPTH + 1) // 2
N_ODD = DEPTH // 2

kernel_name = 'hybrid_mla_moba_deepnorm_adaln_moe'


def layer_norm(x, g, b):
    xf = x.astype(jnp.float32)
    mu = jnp.mean(xf, axis=-1, keepdims=True)
    var = jnp.mean(jnp.square(xf - mu), axis=-1, keepdims=True)
    return ((xf - mu) * lax.rsqrt(var + LN_EPS) * g + b).astype(x.dtype)


def rms_norm(x, g):
    xf = x.astype(jnp.float32)
    ms = jnp.mean(jnp.square(xf), axis=-1, keepdims=True)
    return (xf * lax.rsqrt(ms + RMS_EPS) * g).astype(x.dtype)


def rope_angles(positions, dim):
    inv_freq = ROPE_THETA ** (-jnp.arange(0, dim, 2, dtype=jnp.float32) / dim)
    ang = positions.astype(jnp.float32)[..., None] * inv_freq
    return jnp.cos(ang), jnp.sin(ang)


def apply_rope(x, cos, sin):
    half = x.shape[-1] // 2
    x1, x2 = x[..., :half], x[..., half:]
    cos = cos.astype(x.dtype)
    sin = sin.astype(x.dtype)
    return jnp.concatenate([x1 * cos - x2 * sin, x2 * cos + x1 * sin], axis=-1)


def swiglu(h, w_gate_up, w_down):
    gate, up = jnp.split(h @ w_gate_up, 2, axis=-1)
    return (jax.nn.silu(gate) * up) @ w_down


def mla_attention(q_nope, q_rope, k_nope, k_rope, v):
    B, S, H, _ = q_nope.shape
    scale = (MLA_NOPE + MLA_ROPE) ** -0.5
    key_pos = jnp.arange(S)

    def block(i):
        start = i * ATTN_Q_BLOCK
        qn = lax.dynamic_slice_in_dim(q_nope, start, ATTN_Q_BLOCK, axis=1)
        qr = lax.dynamic_slice_in_dim(q_rope, start, ATTN_Q_BLOCK, axis=1)
        s = (jnp.einsum('bqhd,bkhd->bhqk', qn, k_nope, preferred_element_type=jnp.float32)
             + jnp.einsum('bqhr,bkr->bhqk', qr, k_rope, preferred_element_type=jnp.float32)) * scale
        q_pos = start + jnp.arange(ATTN_Q_BLOCK)
        s = jnp.where(key_pos[None, :] <= q_pos[:, None], s, -jnp.inf)
        p = jax.nn.softmax(s, axis=-1).astype(v.dtype)
        return jnp.einsum('bhqk,bkhd->bqhd', p, v)

    out = lax.map(block, jnp.arange(S // ATTN_Q_BLOCK))
    return out.transpose(1, 0, 2, 3, 4).reshape(B, S, H, MLA_V)


def mla_mixer(u, cos, sin, w_down, q_norm, kv_norm, w_uq, w_ukv, w_o):
    B, S, _ = u.shape
    c_q, c_kv, k_rope = jnp.split(u @ w_down, [MLA_Q_LORA, MLA_Q_LORA + MLA_KV_LORA], axis=-1)
    c_q = rms_norm(c_q, q_norm)
    c_kv = rms_norm(c_kv, kv_norm)
    q = (c_q @ w_uq).reshape(B, S, MLA_HEADS, MLA_NOPE + MLA_ROPE)
    q_nope = q[..., :MLA_NOPE]
    q_rope = apply_rope(q[..., MLA_NOPE:], cos[:, :, None, :], sin[:, :, None, :])
    kv = (c_kv @ w_ukv).reshape(B, S, MLA_HEADS, MLA_NOPE + MLA_V)
    k_nope, v = kv[..., :MLA_NOPE], kv[..., MLA_NOPE:]
    k_rope = apply_rope(k_rope, cos, sin)
    o = mla_attention(q_nope, q_rope, k_nope, k_rope, v)
    return o.reshape(B, S, MLA_HEADS * MLA_V) @ w_o


def moba_attention(q, k, v):
    B, S, H, Dh = q.shape
    nb = -(-S // MOBA_BLOCK)
    pad = nb * MOBA_BLOCK - S
    topk = min(MOBA_TOPK, nb)
    scale = Dh ** -0.5

    def to_blocks(t):
        t = jnp.pad(t, ((0, 0), (0, pad), (0, 0), (0, 0)))
        return t.reshape(B, nb, MOBA_BLOCK, H, Dh).transpose(0, 3, 1, 2, 4)

    kb, vb = to_blocks(k), to_blocks(v)
    k_mean = jnp.mean(kb.astype(jnp.float32), axis=3)
    qh = q.transpose(0, 2, 1, 3)
    blk_ids = jnp.arange(nb)
    b_idx = jnp.arange(B)[:, None, None, None]
    h_idx = jnp.arange(H)[None, :, None, None]

    def chunk(i):
        start = i * MOBA_Q_CHUNK
        qc = lax.dynamic_slice_in_dim(qh, start, MOBA_Q_CHUNK, axis=2)
        q_pos = start + jnp.arange(MOBA_Q_CHUNK)
        own = start // MOBA_BLOCK
        gate = jnp.einsum('bhqd,bhnd->bhqn', qc.astype(jnp.float32), k_mean)
        gate = jnp.where(blk_ids < own, gate, -jnp.inf)
        _, sel = lax.top_k(gate, topk)
        k_sel = kb[b_idx, h_idx, sel]
        v_sel = vb[b_idx, h_idx, sel]
        s_sel = jnp.einsum('bhqd,bhqnkd->bhqnk', qc, k_sel,
                           preferred_element_type=jnp.float32) * scale
        valid = (jnp.arange(topk) < own)[:, None]
        s_sel = jnp.where(valid, s_sel, -jnp.inf).reshape(B, H, MOBA_Q_CHUNK, topk * MOBA_BLOCK)
        k_own = lax.dynamic_index_in_dim(kb, own, axis=2, keepdims=False)
        v_own = lax.dynamic_index_in_dim(vb, own, axis=2, keepdims=False)
        s_own = jnp.einsum('bhqd,bhkd->bhqk', qc, k_own,
                           preferred_element_type=jnp.float32) * scale
        own_pos = own * MOBA_BLOCK + jnp.arange(MOBA_BLOCK)
        s_own = jnp.where(own_pos[None, :] <= q_pos[:, None], s_own, -jnp.inf)
        p = jax.nn.softmax(jnp.concatenate([s_sel, s_own], axis=-1), axis=-1).astype(v.dtype)
        p_sel = p[..., :topk * MOBA_BLOCK].reshape(B, H, MOBA_Q_CHUNK, topk, MOBA_BLOCK)
        p_own = p[..., topk * MOBA_BLOCK:]
        return (jnp.einsum('bhqnk,bhqnkd->bhqd', p_sel, v_sel)
                + jnp.einsum('bhqk,bhkd->bhqd', p_own, v_own))

    out = lax.map(chunk, jnp.arange(S // MOBA_Q_CHUNK))
    return out.transpose(1, 0, 3, 2, 4).reshape(B, S, H, Dh)


def moba_mixer(u, cos, sin, w_qkv, w_o):
    B, S, _ = u.shape
    qkv = (u @ w_qkv).reshape(B, S, 3, MOBA_HEADS, MOBA_HEAD_DIM)
    q, k, v = qkv[:, :, 0], qkv[:, :, 1], qkv[:, :, 2]
    cos4, sin4 = cos[:, :, None, :], sin[:, :, None, :]
    q = jnp.concatenate([apply_rope(q[..., :MOBA_ROT_DIM], cos4, sin4), q[..., MOBA_ROT_DIM:]], axis=-1)
    k = jnp.concatenate([apply_rope(k[..., :MOBA_ROT_DIM], cos4, sin4), k[..., MOBA_ROT_DIM:]], axis=-1)
    o = moba_attention(q, k, v)
    return o.reshape(B, S, MOBA_HEADS * MOBA_HEAD_DIM) @ w_o


def moe_swiglu(u, w_router, e_gate_up, e_down):
    B, S, D = u.shape
    T = B * S
    t = u.reshape(T, D)
    logits = jnp.einsum('td,de->te', t, w_router, preferred_element_type=jnp.float32)
    top_logit, top_idx = lax.top_k(logits, TOP_K)
    top_w = jax.nn.softmax(top_logit, axis=-1)
    A = T * TOP_K
    expert_flat = top_idx.reshape(A)
    token_flat = jnp.repeat(jnp.arange(T, dtype=jnp.int32), TOP_K)
    weight_flat = top_w.reshape(A)
    order = jnp.argsort(expert_flat)
    se, st, sw = expert_flat[order], token_flat[order], weight_flat[order]
    counts = jnp.bincount(expert_flat, length=N_EXPERTS)
    padded = (counts + MOE_ROW_BLOCK - 1) // MOE_ROW_BLOCK * MOE_ROW_BLOCK
    pad_end = jnp.cumsum(padded)
    pad_start = pad_end - padded
    grp_start = jnp.cumsum(counts) - counts
    dest = pad_start[se] + (jnp.arange(A, dtype=jnp.int32) - grp_start[se])
    P = -(-A // MOE_ROW_BLOCK) * MOE_ROW_BLOCK + N_EXPERTS * MOE_ROW_BLOCK
    row_token = jnp.zeros((P,), jnp.int32).at[dest].set(st)
    row_w = jnp.zeros((P,), jnp.float32).at[dest].set(sw)
    n_blk = P // MOE_ROW_BLOCK
    blk_start = jnp.arange(n_blk) * MOE_ROW_BLOCK
    blk_expert = jnp.minimum(jnp.sum(pad_end[None, :] <= blk_start[:, None], axis=1), N_EXPERTS - 1)
    xs = t[row_token].reshape(n_blk, MOE_ROW_BLOCK, D)

    def run(args):
        xb, e = args
        return swiglu(xb, e_gate_up[e], e_down[e])

    ys = lax.map(run, (xs, blk_expert)).reshape(P, D)
    out = jnp.zeros_like(t).at[row_token].add(ys * row_w[:, None].astype(ys.dtype))
    return out.reshape(B, S, D)


def setup_inputs(seed: int = 0) -> dict:
    key = jax.random.key(seed)
    ks = jax.random.split(key, 22)
    D = D_MODEL

    def nrm(k, shape, fan_in, scale=1.0):
        return jax.random.normal(k, shape, jnp.float32) * (scale * fan_in ** -0.5)

    def small(k, shape):
        return 0.01 * jax.random.normal(k, shape, jnp.float32)

    x = jax.random.normal(ks[0], (BATCH, SEQ, D), jnp.float32)
    c = jax.random.normal(ks[1], (BATCH, D), jnp.float32)
    positions = (jax.random.randint(ks[2], (BATCH, 1), 0, 1024, dtype=jnp.int32)
                 + jnp.arange(SEQ, dtype=jnp.int32)[None, :])
    w_ada = nrm(ks[3], (DEPTH, D, 6 * D), D, ADA_SCALE)
    b_ada = small(ks[4], (DEPTH, 6 * D))
    ln_mix_g = 1.0 + small(ks[5], (DEPTH, D))
    ln_mix_b = small(ks[6], (DEPTH, D))
    ln_ffn_g = 1.0 + small(ks[7], (DEPTH, D))
    ln_ffn_b = small(ks[8], (DEPTH, D))
    mla_w_down = nrm(ks[9], (N_EVEN, D, MLA_Q_LORA + MLA_KV_LORA + MLA_ROPE), D)
    mla_q_norm = 1.0 + small(ks[10], (N_EVEN, MLA_Q_LORA))
    mla_kv_norm = 1.0 + small(ks[11], (N_EVEN, MLA_KV_LORA))
    mla_w_uq = nrm(ks[12], (N_EVEN, MLA_Q_LORA, MLA_HEADS * (MLA_NOPE + MLA_ROPE)), MLA_Q_LORA)
    mla_w_ukv = nrm(ks[13], (N_EVEN, MLA_KV_LORA, MLA_HEADS * (MLA_NOPE + MLA_V)), MLA_KV_LORA)
    mla_w_o = nrm(ks[14], (N_EVEN, MLA_HEADS * MLA_V, D), MLA_HEADS * MLA_V, DEEPNORM_BETA)
    moba_w_qkv = nrm(ks[15], (N_ODD, D, 3 * MOBA_HEADS * MOBA_HEAD_DIM), D)
    moba_w_o = nrm(ks[16], (N_ODD, MOBA_HEADS * MOBA_HEAD_DIM, D), MOBA_HEADS * MOBA_HEAD_DIM, DEEPNORM_BETA)
    ffn_w_gate_up = nrm(ks[17], (N_EVEN, D, 2 * D_FF), D)
    ffn_w_down = nrm(ks[18], (N_EVEN, D_FF, D), D_FF, DEEPNORM_BETA)
    moe_w_router = nrm(ks[19], (N_ODD, D, N_EXPERTS), D)
    moe_w_gate_up = nrm(ks[20], (N_ODD, N_EXPERTS, D, 2 * D_FF), D)
    moe_w_down = nrm(ks[21], (N_ODD, N_EXPERTS, D_FF, D), D_FF, DEEPNORM_BETA)
    return {'x': x, 'c': c, 'positions': positions, 'w_ada': w_ada, 'b_ada': b_ada,
            'ln_mix_g': ln_mix_g, 'ln_mix_b': ln_mix_b, 'ln_ffn_g': ln_ffn_g, 'ln_ffn_b': ln_ffn_b,
            'mla_w_down': mla_w_down, 'mla_q_norm': mla_q_norm, 'mla_kv_norm': mla_kv_norm,
            'mla_w_uq': mla_w_uq, 'mla_w_ukv': mla_w_ukv, 'mla_w_o': mla_w_o,
            'moba_w_qkv': moba_w_qkv, 'moba_w_o': moba_w_o,
            'ffn_w_gate_up': ffn_w_gate_up, 'ffn_w_down': ffn_w_down,
            'moe_w_router': moe_w_router, 'moe_w_gate_up': moe_w_gate_up, 'moe_w_down': moe_w_down}


def reference(x, c, positions, w_ada, b_ada, ln_mix_g, ln_mix_b, ln_ffn_g, ln_ffn_b,
              mla_w_down, mla_q_norm, mla_kv_norm, mla_w_uq, mla_w_ukv, mla_w_o,
              moba_w_qkv, moba_w_o, ffn_w_gate_up, ffn_w_down,
              moe_w_router, moe_w_gate_up, moe_w_down):
    cos_mla, sin_mla = rope_angles(positions, MLA_ROPE)
    cos_moba, sin_moba = rope_angles(positions, MOBA_ROT_DIM)
    c_act = jax.nn.silu(c)
    for l in range(DEPTH):
        j = l // 2
        ada = c_act @ w_ada[l] + b_ada[l]
        sh_m, sc_m, g_m, sh_f, sc_f, g_f = jnp.split(ada[:, None, :], 6, axis=-1)
        u = x * (1.0 + sc_m) + sh_m
        if l % N_MIXERS == 0:
            y = mla_mixer(u, cos_mla, sin_mla, mla_w_down[j], mla_q_norm[j], mla_kv_norm[j],
                          mla_w_uq[j], mla_w_ukv[j], mla_w_o[j])
        else:
            y = moba_mixer(u, cos_moba, sin_moba, moba_w_qkv[j], moba_w_o[j])
        x = layer_norm(DEEPNORM_ALPHA * x + (1.0 + g_m) * y, ln_mix_g[l], ln_mix_b[l])
        u = x * (1.0 + sc_f) + sh_f
        if l % 2 == 0:
            y = swiglu(u, ffn_w_gate_up[j], ffn_w_down[j])
        else:
            y = moe_swiglu(u, moe_w_router[j], moe_w_gate_up[j], moe_w_down[j])
        x = layer_norm(DEEPNORM_ALPHA * x + (1.0 + g_f) * y, ln_ffn_g[l], ln_ffn_b[l])
    return x
```

```python
import math
import os
import re
from contextlib import ExitStack
import numpy as np
import ml_dtypes
import concourse.bass as bass
import concourse.mybir as mybir
from concourse.bass_utils import run_bass_kernel_spmd

F32 = mybir.dt.float32
BF16 = mybir.dt.bfloat16
I32 = mybir.dt.int32
U8 = mybir.dt.uint8
ALU = mybir.AluOpType
AF = mybir.ActivationFunctionType
AX = mybir.AxisListType

T = 2048
D = 2048
NFB = 16
TC = 512
NCH = 4
NT = 16
DEPTH = 4
DFF = 5632
NKF = 44
NE = 8
CAP = 768
NST = 6
ALPHA = (2.0 * DEPTH) ** 0.25
LN_EPS = 1e-5
RMS_EPS = 1e-6
THETA = 500000.0
NEGB = -30000.0
NCORES = 4
RING_ELEMS = 12288
SEM_LIMIT = 28000

ENGS = ("sp", "act", "dve", "pool", "pe")


class Prog:
    def __init__(self, nc):
        self.nc = nc
        self.ops = {e: [] for e in ENGS}
        self.epoch = {}
        self.cnt = {}
        self.seen = {e: {} for e in ENGS}
        self.lastw = {}
        self.readers = {}
        self.ring_i = 0
        self.isdma = {}

    def _bump(self, stream, inc):
        ep = self.epoch.get(stream, 0)
        c = self.cnt.get((stream, ep), 0)
        if c + inc > SEM_LIMIT:
            ep += 1
            self.epoch[stream] = ep
            c = 0
        c += inc
        self.cnt[(stream, ep)] = c
        return (stream, ep, c)

    def op(self, eng, fn, reads=(), writes=(), dma=None):
        stream = dma if dma else eng
        self.isdma[stream] = bool(dma)
        waits = {}

        def need(tok):
            s, ep, v = tok
            if s == eng and not dma and eng == "pe":
                return
            if dma and s == stream:
                return
            k = (s, ep)
            if self.seen[eng].get(k, 0) >= v:
                return
            if waits.get(k, 0) < v:
                waits[k] = v

        for key in reads:
            lw = self.lastw.get(key)
            if lw is not None:
                need(lw)
        for key in writes:
            lw = self.lastw.get(key)
            if lw is not None:
                need(lw)
            for tok in self.readers.get(key, {}).values():
                need(tok)
        for k, v in waits.items():
            self.seen[eng][k] = v
        tok = self._bump(stream, 16 if dma else 1)
        for key in writes:
            self.lastw[key] = tok
            self.readers[key] = {}
        for key in reads:
            self.readers.setdefault(key, {})[(tok[0], tok[1])] = tok
        self.ops[eng].append((list(waits.items()), fn, (stream, tok[1]), 16 if dma else 1))
        return tok

    def barrier(self, engines=("sp", "act", "dve", "pe")):
        toks = []
        for (s, ep), c in self.cnt.items():
            toks.append((s, ep, c))
        for eng in engines:
            waits = {}
            for (s, ep, v) in toks:
                if s == eng:
                    continue
                if s.startswith("pool"):
                    continue
                k = (s, ep)
                if self.seen[eng].get(k, 0) >= v:
                    continue
                waits[k] = v
                self.seen[eng][k] = v
            if waits:
                self.ops[eng].append((list(waits.items()), None, None, 0))

    def emit(self, es):
        nc = self.nc
        sems = {}
        for (s, ep) in self.cnt.keys():
            sems[(s, ep)] = es.enter_context(nc.semaphore("s_%s_%d" % (s, ep)))
        block = es.enter_context(nc.Block())
        ops = self.ops

        def run(e, lst):
            for waits, fn, sig, inc in lst:
                for k, v in waits:
                    e.wait_ge(sems[k], v)
                if fn is None:
                    continue
                ins = fn(e)
                ins.then_inc(sems[sig], inc)

        @block.sync
        def _(e):
            run(e, ops["sp"])

        @block.scalar
        def _(e):
            run(e, ops["act"])

        @block.vector
        def _(e):
            run(e, ops["dve"])

        @block.gpsimd
        def _(e):
            run(e, ops["pool"])

        @block.tensor
        def _(e):
            run(e, ops["pe"])


def build(nsub=8, dbg=False):
    nc = bass.Bass("TRN2", target_bir_lowering=False)
    P = Prog(nc)

    def din(name, shape, dt=F32):
        return nc.dram_tensor(name, list(shape), dt, kind="ExternalInput")

    def dscr(name, shape, dt=F32):
        return nc.dram_tensor(name, list(shape), dt, kind=("ExternalOutput" if dbg else "Internal"))

    nlayers = (nsub + 1) // 2
    xT_in = din("xT", [NFB, 128, T])
    pos_in = din("pos", [1, T], I32)
    cstf_in = din("cstf", [128, 1024])
    cstb_in = din("cstb", [128, 1024], BF16)
    vecs_in = din("vecs", [128, 512])
    w_ada = din("w_ada", [DEPTH, D, 6 * D])
    mla_w_down = din("mla_w_down", [2, D, 1088])
    mla_w_uq = din("mla_w_uq", [2, 512, 3072])
    mla_w_ukv = din("mla_w_ukv", [2, 512, 4096])
    mla_w_o = din("mla_w_o", [2, D, D])
    if nsub > 1:
        ffn_w_gate_up = din("ffn_w_gate_up", [2, D, 2 * DFF])
        ffn_w_down = din("ffn_w_down", [2, DFF, D])
    if nsub > 2:
        moba_w_qkv = din("moba_w_qkv", [2, D, 6144])
        moba_w_o = din("moba_w_o", [2, D, D])
    if nsub > 3:
        moe_w_router = din("moe_w_router", [2, D, NE])
        moe_w_gate_up = din("moe_w_gate_up", [2, NE, D, 2 * DFF])
        moe_w_down = din("moe_w_down", [2, NE, DFF, D])
    outT = nc.dram_tensor("outT", [NFB, 128, T], F32, kind="ExternalOutput")

    xS = dscr("xS", [NFB, 128, T])
    yS = dscr("yS", [NFB, 128, T])
    tab = dscr("tab", [4, 64, T])
    qn_d = dscr("qn_d", [16, 128, T], BF16)
    kn_d = dscr("kn_d", [16, 128, T], BF16)
    qr_d = dscr("qr_d", [16, 64, T], BF16)
    v_d = dscr("v_d", [T, 2048], BF16)
    ut_d = dscr("ut_d", [T, 2048], BF16)
    ys_d = dscr("ys_d", [NE, CAP, 2048], BF16)
    ptw_d = dscr("ptw_d", [NE, NST, 128, T], BF16)

    ring = [nc.alloc_sbuf_tensor("ring%d" % i, [128, RING_ELEMS], BF16) for i in range(2)]
    cstf = nc.alloc_sbuf_tensor("cstf_sb", [128, 1024], F32)
    cstb = nc.alloc_sbuf_tensor("cstb_sb", [128, 1024], BF16)
    vecs = nc.alloc_sbuf_tensor("vecs_sb", [128, 512], F32)
    ADA = nc.alloc_sbuf_tensor("ADA", [128, 4 * 96], F32)
    PV = nc.alloc_sbuf_tensor("PVEC", [128, 64], F32)
    cact = nc.alloc_sbuf_tensor("cact", [128, 16], BF16)
    SM = nc.alloc_sbuf_tensor("SM", [128, 64], F32)
    RT = nc.alloc_sbuf_tensor("RT", [128, 16 * 8 * 4], F32)
    RTB = nc.alloc_sbuf_tensor("RTB", [128, 16 * 8], BF16)
    WR = nc.alloc_sbuf_tensor("WR", [128, 16 * 8], BF16)
    LNP = nc.alloc_sbuf_tensor("LNP", [128, 256], F32)
    remaining = nc.sbuf_bytes_remaining
    ARENA_BYTES = (remaining // 64) * 64 - 256
    arena = nc.alloc_sbuf_tensor("arena", [128, ARENA_BYTES], U8)
    ps_all = nc.alloc_psum_tensor("ps_all", [128, 4096], F32)
    ps_bf = ps_all[:, :].bitcast(BF16)

    ident_f = cstf[:, 0:128]
    iota_s = cstf[:, 128:128 + CAP]
    ownmask = vecs[:, 416:480]
    invf_mla = vecs[:, 480:481]
    invf_moba = vecs[:, 481:482]
    sgn_mla = vecs[:, 482:483]
    sgn_moba = vecs[:, 483:484]
    ones_f = cstf[:, 896:1024]
    ident_b = cstb[:, 0:128]
    trimask = cstb[:, 128:256]
    ltri = cstb[:, 256:384]
    ones_b = cstb[:, 384:512]
    V_BADA = 0
    V_LNP = 384

    aoff = [0]

    def carve(nbytes):
        o = aoff[0]
        nb = (nbytes + 63) // 64 * 64
        assert o + nb <= ARENA_BYTES, ("arena overflow", o, nb, ARENA_BYTES)
        aoff[0] = o + nb
        return arena[:, o:o + nbytes]

    def carve_t(shape_free, dt):
        n = int(np.prod(shape_free))
        esz = 4 if dt == F32 or dt == I32 else 2
        v = carve(n * esz).bitcast(dt)
        return v

    def arena_reset():
        aoff[0] = 0

    def bank(i):
        return ps_all[:, i * 512:(i + 1) * 512]

    def _sname(key):
        return "d_" + re.sub(r"[^A-Za-z0-9]", "_", str(key))

    def dma_sp(out, in_, reads, writes, stream="sp_ld"):
        key = reads[0] if stream == "sp_st" else writes[0]
        P.op("sp", lambda e: e.dma_start(out=out, in_=in_), reads, writes, dma=_sname(key))

    def wdma(out, in_, rkey):
        P.op("pool", lambda e: e.dma_start(out=out, in_=in_), (), (rkey,), dma="pool_" + _sname(rkey))

    def ring_next():
        i = P.ring_i
        P.ring_i = 1 - i
        return i, ("ring", i)

    def pe_acc(out_ap, pairs, reads, writes):
        def fn(e):
            n = len(pairs)
            ins = None
            for i, (l, r) in enumerate(pairs):
                ins = e.matmul(out_ap, lhsT=l, rhs=r, start=(i == 0), stop=(i == n - 1))
            return ins
        P.op("pe", fn, reads, writes)

    def act_copy(out, in_, reads, writes):
        P.op("act", lambda e: e.copy(out, in_), reads, writes)

    def dve(fn, reads, writes):
        P.op("dve", fn, reads, writes)

    def act(fn, reads, writes):
        P.op("act", fn, reads, writes)

    dma_sp(cstf[:, :], cstf_in[:, :], (), ("cstf",))
    dma_sp(cstb[:, :], cstb_in[:, :], (), ("cstb",))
    dma_sp(vecs[:, :], vecs_in[:, :], (), ("vecs",))
    lnp_in = din("lnp", [128, 256])
    dma_sp(LNP[:, :], lnp_in[:, :], (), ("lnp",))
    cT = vecs[:, 384:400]
    qkvn = vecs[:, 400:416]
    act(lambda e: e.activation(cact[:, :], cT, AF.Silu), ("vecs",), ("cact",))

    for l in range(nlayers):
        for jt in range(24):
            ri, rk = ring_next()
            wt = ring[ri][:, 0:16 * 512].rearrange("p (k n) -> p k n", k=16)
            src = w_ada[l].rearrange("(k p) n -> p k n", p=128)
            wdma(wt, src[:, :, jt * 512:(jt + 1) * 512], rk)

            def fn(e, wt=wt, jt=jt):
                ins = None
                for blk in range(4):
                    col = jt * 4 + blk
                    for k in range(16):
                        ins = e.matmul(ps_all[:, col:col + 1], lhsT=wt[:, k, blk * 128:(blk + 1) * 128],
                                       rhs=cact[:, k:k + 1], start=(k == 0), stop=(k == 15))
                return ins
            P.op("pe", fn, (rk, "cact"), (("ps", 0),))
        dve(lambda e, l=l: e.tensor_tensor(ADA[:, l * 96:(l + 1) * 96], ps_all[:, 0:96],
                                           vecs[:, l * 96:(l + 1) * 96], op=ALU.add),
            (("ps", 0), "vecs"), ("ADA",))

    def ada(l, idx):
        return ADA[:, l * 96 + idx * 16: l * 96 + (idx + 1) * 16]

    def lnp(l, idx):
        return LNP[:, (l * 4 + idx) * 16:(l * 4 + idx + 1) * 16]

    arena_reset()
    posi = carve_t([T], I32)
    posf = carve_t([T], F32)
    ang = carve_t([T], F32)
    tmpa = carve_t([T], F32)
    tmps = carve_t([T], F32)
    dma_sp(posi[0:64, :], pos_in[0, :].partition_broadcast(64), (), ("posi",))
    dve(lambda e: e.tensor_copy(posf[0:64, :], posi[0:64, :]), ("posi",), ("posf",))
    tmpi = carve_t([T], I32)
    TWO_PI = 2.0 * math.pi

    def range_reduce(npart, shift):
        dve(lambda e: e.tensor_scalar(tmpa[0:npart, :], ang[0:npart, :], shift, 1.0 / TWO_PI, op0=ALU.add, op1=ALU.mult),
            ("ang", "tmps"), ("tmpa",))
        dve(lambda e: e.tensor_copy(tmpi[0:npart, :], tmpa[0:npart, :]), ("tmpa",), ("tmpi",))
        dve(lambda e: e.tensor_copy(tmpa[0:npart, :], tmpi[0:npart, :]), ("tmpi",), ("tmpa",))
        dve(lambda e: e.scalar_tensor_tensor(tmpa[0:npart, :], tmpa[0:npart, :], -TWO_PI, ang[0:npart, :],
                                             op0=ALU.mult, op1=ALU.add), ("tmpa", "ang"), ("tmpa",))
        dve(lambda e: e.tensor_scalar_add(tmpa[0:npart, :], tmpa[0:npart, :], shift), ("tmpa",), ("tmpa",))
        dve(lambda e: e.tensor_scalar(tmps[0:npart, :], tmpa[0:npart, :], math.pi, TWO_PI, op0=ALU.is_gt, op1=ALU.mult),
            ("tmpa",), ("tmps",))
        dve(lambda e: e.tensor_tensor(tmpa[0:npart, :], tmpa[0:npart, :], tmps[0:npart, :], op=ALU.subtract),
            ("tmpa", "tmps"), ("tmpa",))
        dve(lambda e: e.tensor_scalar(tmps[0:npart, :], tmpa[0:npart, :], -math.pi, TWO_PI, op0=ALU.is_lt, op1=ALU.mult),
            ("tmpa",), ("tmps",))
        dve(lambda e: e.tensor_tensor(tmpa[0:npart, :], tmpa[0:npart, :], tmps[0:npart, :], op=ALU.add),
            ("tmpa", "tmps"), ("tmpa",))
        dve(lambda e: e.tensor_scalar(tmpa[0:npart, :], tmpa[0:npart, :], math.pi, -math.pi, op0=ALU.min, op1=ALU.max),
            ("tmpa",), ("tmpa",))

    for ti, (npart, invf, sgn) in enumerate(((64, invf_mla, sgn_mla), (32, invf_moba, sgn_moba))):
        dve(lambda e, npart=npart, invf=invf: e.tensor_scalar(ang[0:npart, :], posf[0:npart, :], invf[0:npart, :], None,
                                                              op0=ALU.mult), ("posf", "vecs", "tmpa"), ("ang",))
        range_reduce(npart, 0.5 * math.pi)
        act(lambda e, npart=npart: e.activation(tmps[0:npart, :], tmpa[0:npart, :], AF.Sin), ("tmpa",), ("tmps",))
        dma_sp(tab[2 * ti, 0:npart, :], tmps[0:npart, :], ("tmps",), (("tab", 2 * ti),), stream="sp_st")
        range_reduce(npart, 0.0)
        act(lambda e, npart=npart: e.activation(tmps[0:npart, :], tmpa[0:npart, :], AF.Sin), ("tmpa",), ("tmps",))
        dve(lambda e, npart=npart, sgn=sgn: e.tensor_scalar(tmps[0:npart, :], tmps[0:npart, :], sgn[0:npart, :], None,
                                                            op0=ALU.mult), ("tmps", "vecs"), ("tmps",))
        dma_sp(tab[2 * ti + 1, 0:npart, :], tmps[0:npart, :], ("tmps",), (("tab", 2 * ti + 1),), stream="sp_st")
    P.barrier()

    arena_reset()
    R1 = carve_t([NFB, T], BF16).rearrange("p (k n) -> p k n", k=NFB)
    A_BASE = aoff[0]

    def phase_reset():
        aoff[0] = A_BASE

    dump_i = [0]

    def dump_r1(name, keys):
        if not dbg:
            return
        dt_ = nc.dram_tensor("dbg_" + name, [128, NFB * T], BF16, kind="ExternalOutput")
        P.op("sp", lambda e: e.dma_start(out=dt_[:, :], in_=R1[:, :, :].rearrange("p k n -> p (k n)")), keys, (), dma="d_dump%d" % dump_i[0])
        dump_i[0] += 1

    def ln_phase(l, sub, first, last):
        phase_reset()
        XI = [carve_t([TC], F32) for _ in range(3)]
        if first:
            sc, sh = ada(0, 1), ada(0, 0)
            dve(lambda e: e.tensor_scalar_add(PV[:, 0:16], sc, 1.0), ("ADA",), ("PV",))
            for n in range(NCH):
                for fb in range(NFB):
                    xi = XI[(n * NFB + fb) % 3]
                    kx = ("XI", (n * NFB + fb) % 3)
                    dma_sp(xi, xT_in[fb, :, n * TC:(n + 1) * TC], (), (kx,))
                    dve(lambda e, xi=xi, fb=fb, n=n: e.tensor_scalar(
                        R1[:, fb, n * TC:(n + 1) * TC], xi, PV[:, fb:fb + 1], sh[:, fb:fb + 1],
                        op0=ALU.mult, op1=ALU.add), (kx, "PV", "ADA"), (("R1", n),))
            dump_r1("u0", tuple(("R1", n) for n in range(NCH)))
            P.barrier()
            return
        xsrc = xT_in if (l == 0 and sub == 0) else xS
        xdst = outT if last else xS
        g = ada(l, 2 + 3 * sub)
        gam, bet = lnp(l, 2 * sub), lnp(l, 2 * sub + 1)
        dve(lambda e: e.tensor_scalar_add(PV[:, 0:16], g, 1.0), ("ADA",), ("PV",))
        if not last:
            if sub == 0:
                sc, sh = ada(l, 4), ada(l, 3)
            else:
                sc, sh = ada(l + 1, 1), ada(l + 1, 0)
            dve(lambda e: e.tensor_scalar_add(PV[:, 48:64], sc, 1.0), ("ADA", "PV"), ("PV",))
            dve(lambda e: e.tensor_tensor(PV[:, 16:32], gam, PV[:, 48:64], op=ALU.mult), ("lnp", "PV"), ("PV",))
            dve(lambda e: e.tensor_tensor(PV[:, 32:48], bet, PV[:, 48:64], op=ALU.mult), ("lnp", "PV"), ("PV",))
            dve(lambda e: e.tensor_tensor(PV[:, 32:48], PV[:, 32:48], sh, op=ALU.add), ("PV", "ADA"), ("PV",))
        YI = [carve_t([TC], F32) for _ in range(3)]
        ZB = carve_t([NFB, TC], F32).rearrange("p (k n) -> p k n", k=NFB)
        SQ = [carve_t([TC], F32) for _ in range(2)]
        MB = carve_t([TC], F32)
        RB = carve_t([TC], F32)
        TMP = carve_t([TC], F32)
        XO = [carve_t([TC], F32) for _ in range(3)]
        cnt = 0
        for n in range(NCH):
            cs = slice(n * TC, (n + 1) * TC)
            for fb in range(NFB):
                b3 = cnt % 3
                b2 = cnt % 2
                cnt += 1
                xi, yi, sq = XI[b3], YI[b3], SQ[b2]
                kx, ky, ksq = ("XI", b3), ("YI", b3), ("SQ", b2)
                dma_sp(xi, xsrc[fb, :, cs], (("xS", n, fb),), (kx,))
                dma_sp(yi, yS[fb, :, cs], (("yS", n, fb),), (ky,))
                dve(lambda e, yi=yi, fb=fb: e.tensor_scalar(yi, yi, PV[:, fb:fb + 1], None, op0=ALU.mult),
                    (ky, "PV"), (ky,))
                dve(lambda e, xi=xi, yi=yi, fb=fb: e.scalar_tensor_tensor(ZB[:, fb, :], xi, ALPHA, yi,
                                                                          op0=ALU.mult, op1=ALU.add),
                    (kx, ky), (("ZB", fb),))
                act(lambda e, sq=sq, fb=fb: e.activation(sq, ZB[:, fb, :], AF.Square), (("ZB", fb),), (ksq,))
                P.op("pe", lambda e, fb=fb: e.matmul(bank(0), lhsT=ones_f, rhs=ZB[:, fb, :], start=(fb == 0),
                                                     stop=(fb == NFB - 1)), (("ZB", fb), "cstf"), (("ps", 0),))
                P.op("pe", lambda e, fb=fb, sq=sq: e.matmul(bank(1), lhsT=ones_f, rhs=sq, start=(fb == 0),
                                                            stop=(fb == NFB - 1)), (ksq, "cstf"), (("ps", 1),))
            dve(lambda e: e.tensor_scalar(MB, bank(0), 1.0 / D, None, op0=ALU.mult), (("ps", 0),), ("MB",))
            dve(lambda e: e.tensor_tensor(TMP, MB, MB, op=ALU.mult), ("MB",), ("TMP",))
            dve(lambda e: e.scalar_tensor_tensor(RB, bank(1), 1.0 / D, TMP, op0=ALU.mult, op1=ALU.subtract),
                (("ps", 1), "TMP"), ("RB",))
            dve(lambda e: e.tensor_scalar_add(RB, RB, LN_EPS), ("RB",), ("RB",))
            act(lambda e: e.activation(RB, RB, AF.Sqrt), ("RB",), ("RB",))
            dve(lambda e: e.reciprocal(RB, RB), ("RB",), ("RB",))
            for fb in range(NFB):
                b3 = fb % 3
                xo = XO[b3]
                kxo = ("XO", b3)
                dve(lambda e, fb=fb: e.tensor_tensor(ZB[:, fb, :], ZB[:, fb, :], MB, op=ALU.subtract),
                    (("ZB", fb), "MB"), (("ZB", fb),))
                dve(lambda e, fb=fb: e.tensor_tensor(ZB[:, fb, :], ZB[:, fb, :], RB, op=ALU.mult),
                    (("ZB", fb), "RB"), (("ZB", fb),))
                act(lambda e, fb=fb, xo=xo: e.activation(xo, ZB[:, fb, :], AF.Identity, bias=bet[:, fb:fb + 1],
                                                         scale=gam[:, fb:fb + 1]), (("ZB", fb), "lnp"), (kxo,))
                dma_sp(xdst[fb, :, cs], xo, (kxo,), (("xS", n, fb),), stream="sp_st")
                if not last:
                    act(lambda e, fb=fb, cs=cs: e.activation(R1[:, fb, cs], ZB[:, fb, :], AF.Identity,
                                                             bias=PV[:, 32 + fb:33 + fb], scale=PV[:, 16 + fb:17 + fb]),
                        (("ZB", fb), "PV"), (("R1", n),))
        P.barrier()

    def attention(kind, nheads=16):
        scale = (192.0 ** -0.5) if kind == "mla" else (128.0 ** -0.5)
        QN = [carve_t([T], BF16) for _ in range(2)]
        KN = [carve_t([T], BF16) for _ in range(2)]
        QR = [carve_t([T], BF16) for _ in range(2)] if kind == "mla" else None
        VH = [carve_t([16 * 128], BF16).rearrange("p (t d) -> p t d", t=16) for _ in range(2)]
        PB = [carve_t([T], BF16) for _ in range(2)]
        PT = [carve_t([16 * 128], BF16).rearrange("p (t d) -> p t d", t=16) for _ in range(2)]
        DG = [carve_t([128], BF16) for _ in range(2)]
        KM = [carve_t([8], BF16) for _ in range(2)]
        KM32 = carve_t([8], F32)
        nogate = bool(os.environ.get("K_NOGATE"))
        kr_b = KR if kind == "mla" else None

        def loads(h):
            hb = h % 2
            dma_sp(QN[hb], qn_d[h, :, :], (("qn_d", h),), (("QN", hb),))
            dma_sp(KN[hb], kn_d[h, :, :], (("kn_d", h),), (("KN", hb),))
            dma_sp(VH[hb], v_d[:, h * 128:(h + 1) * 128].rearrange("(t p) d -> p t d", p=128),
                   tuple(("v_d", t_, h // 4) for t_ in range(NT)), (("VH", hb),))
            if kind == "mla":
                dma_sp(QR[hb][0:64, :], qr_d[h, :, :], (("qr_d", h),), (("QR", hb),))
            elif not nogate:
                kn = KN[hb]
                dve(lambda e, kn=kn: e.tensor_reduce(KM32, kn.rearrange("p (b k) -> p b k", b=8), axis=AX.X, op=ALU.add),
                    (("KN", hb),), ("KM32",))
                dve(lambda e, hb=hb: e.tensor_scalar(KM[hb], KM32, 1.0 / 256.0, None, op0=ALU.mult), ("KM32",), (("KM", hb),))

        def geom(t):
            h, i = divmod(t, NT)
            nk = (i + 1) * 128
            nchk = (nk + 511) // 512
            own = i // 2
            gated = (kind == "moba" and own >= 4 and not nogate)
            return h, i, nk, nchk, own, gated

        def emit_S(t):
            h, i, nk, nchk, own, gated = geom(t)
            hb = h % 2
            qs = slice(i * 128, (i + 1) * 128)
            qn, kn = QN[hb], KN[hb]
            qr_b = QR[hb] if kind == "mla" else None
            rd = [("QN", hb), ("KN", hb), "cstb"]
            if kind == "mla":
                rd += [("QR", hb), "KR"]

            def s_fn(e):
                ins = None
                for c in range(nchk):
                    w = min(512, nk - c * 512)
                    ks = slice(c * 512, c * 512 + w)
                    lastc = (c == nchk - 1)
                    e.matmul(ps_all[:, ks], lhsT=qn[:, qs], rhs=kn[:, ks], start=True,
                             stop=(kind != "mla" and not lastc))
                    if kind == "mla":
                        ins = e.matmul(ps_all[:, ks], lhsT=qr_b[0:64, qs], rhs=kr_b[0:64, ks], start=False,
                                       stop=(not lastc))
                    if lastc:
                        ins = e.matmul(ps_all[:, i * 128:(i + 1) * 128], lhsT=ident_b, rhs=trimask,
                                       start=False, stop=True)
                return ins
            P.op("pe", s_fn, rd, [("ps", c) for c in range(nchk)])
            if gated:
                P.op("pe", lambda e: e.matmul(ps_all[:, 7 * 512:7 * 512 + 8], lhsT=qn[:, qs], rhs=KM[hb],
                                              start=True, stop=True), (("QN", hb), ("KM", hb)), (("ps", 7),))

        def emit_softmax(t):
            h, i, nk, nchk, own, gated = geom(t)
            b2 = t % 2
            pb, dg = PB[b2], DG[b2]
            kpb, kdg = ("PB", b2), ("DG", b2)
            if gated:
                dve(lambda e: e.tensor_tensor(SM[:, 16:24], ps_all[:, 7 * 512:7 * 512 + 8],
                                              ownmask[:, own * 8:(own + 1) * 8], op=ALU.add),
                    (("ps", 7), "vecs"), ("SMg",))
                dve(lambda e: e.max(SM[:, 24:32], SM[:, 16:24]), ("SMg",), ("SMg",))
                dve(lambda e: e.tensor_scalar(SM[:, 32:40], SM[:, 16:24], SM[:, 26:27], None, op0=ALU.is_ge),
                    ("SMg",), ("SMg",))
                dve(lambda e: e.tensor_scalar(SM[:, 32:40], SM[:, 32:40], -1.0, -NEGB, op0=ALU.add, op1=ALU.mult),
                    ("SMg",), ("SMg",))
            pskeys = tuple(("ps", c) for c in range(nchk))
            dve(lambda e: e.reduce_max(SM[:, 4:5], ps_all[:, 0:nk], axis=AX.X), pskeys, ("SMm",))
            dve(lambda e: e.tensor_scalar(SM[:, 5:6], SM[:, 4:5], -scale, None, op0=ALU.mult), ("SMm",), ("SMm",))
            if gated:
                dve(lambda e: e.tensor_scalar(SM[:, 40:48], SM[:, 32:40], SM[:, 5:6], None, op0=ALU.add),
                    ("SMm", "SMg"), ("SMb",))
                nblk = own + 1
                for n in range(nblk):
                    k0 = n * 256
                    w = min(256, nk - k0)
                    bias_ap = SM[:, 40 + n:41 + n] if n < own else SM[:, 5:6]
                    act(lambda e, k0=k0, w=w, bias_ap=bias_ap, n=n: e.activation(
                        pb[:, k0:k0 + w], ps_all[:, k0:k0 + w], AF.Exp, bias=bias_ap, scale=scale,
                        accum_out=SM[:, 48 + n:49 + n]),
                        (("ps", k0 // 512), "SMb", "SMm"), (kpb, "SMs"))
                dve(lambda e: e.reduce_sum(SM[:, 6:7], SM[:, 48:48 + nblk], axis=AX.X), ("SMs",), ("SMr",))
            else:
                act(lambda e: e.activation(pb[:, 0:nk], ps_all[:, 0:nk], AF.Exp, bias=SM[:, 5:6],
                                           scale=scale, accum_out=SM[:, 6:7]),
                    pskeys + ("SMm",), (kpb, "SMr"))
            dve(lambda e: e.reciprocal(SM[:, 7:8], SM[:, 6:7]), ("SMr",), ("SMr",))
            dve(lambda e: e.tensor_scalar(dg, ident_f, SM[:, 7:8], None, op0=ALU.mult), ("SMr", "cstf"), (kdg,))

        def emit_PV(t):
            h, i, nk, nchk, own, gated = geom(t)
            hb = h % 2
            b2 = t % 2
            qs = slice(i * 128, (i + 1) * 128)
            pb, pt, dg, vh = PB[b2], PT[b2], DG[b2], VH[hb]
            kpb, kpt, kdg = ("PB", b2), ("PT", b2), ("DG", b2)
            ngrp = (i + 1 + 3) // 4
            for gI in range(ngrp):
                bk = 4 + (gI % 2)
                nb_ = min(4, i + 1 - gI * 4)

                def t_fn(e, gI=gI, nb_=nb_, bk=bk):
                    ins = None
                    for j in range(nb_):
                        kb = gI * 4 + j
                        ins = e.matmul(ps_all[:, bk * 512 + j * 128: bk * 512 + (j + 1) * 128],
                                       lhsT=pb[:, kb * 128:(kb + 1) * 128], rhs=dg, start=True, stop=True)
                    return ins
                P.op("pe", t_fn, (kpb, kdg), (("ps", bk),))
                act_copy(pt[:, gI * 4:gI * 4 + nb_, :],
                         ps_all[:, bk * 512: bk * 512 + nb_ * 128].rearrange("p (j q) -> p j q", j=nb_),
                         (("ps", bk),), (kpt,))
            pe_acc(ps_all[:, 6 * 512:6 * 512 + 128], [(vh[:, kb, :], pt[:, kb, :]) for kb in range(i + 1)],
                   (("VH", hb), kpt), (("ps", 6),))
            act_copy(R1[:, h, qs], ps_all[:, 6 * 512:6 * 512 + 128], (("ps", 6),), (("R1o", h),))

        ntile = nheads * NT
        loads(0)
        emit_S(0)
        for t in range(ntile):
            if t % NT == 0 and t // NT + 1 < nheads:
                loads(t // NT + 1)
            emit_softmax(t)
            if t + 1 < ntile:
                emit_S(t + 1)
            emit_PV(t)

    def out_proj(w):
        YST = [carve_t([TC], F32) for _ in range(3)]
        src = w.rearrange("(k p) n -> p k n", p=128)
        cnt = 0
        for ct in range(4):
            ri, rk = ring_next()
            wt = ring[ri][:, 0:16 * 512].rearrange("p (k n) -> p k n", k=16)
            wdma(wt, src[:, :, ct * 512:(ct + 1) * 512], rk)
            for mb in range(4):
                m = ct * 4 + mb
                for n in range(NCH):
                    cs = slice(n * TC, (n + 1) * TC)
                    bk = cnt % 8
                    b3 = cnt % 3
                    cnt += 1
                    pe_acc(bank(bk), [(wt[:, k, mb * 128:(mb + 1) * 128], R1[:, k, cs]) for k in range(16)],
                           (rk,) + tuple(("R1o", k) for k in range(16)), (("ps", bk),))
                    act_copy(YST[b3], bank(bk), (("ps", bk),), (("YST", b3),))
                    dma_sp(yS[m, :, cs], YST[b3], (("YST", b3),), (("yS", n, m),), stream="sp_st")

    def mla_mixer(j):
        phase_reset()
        nonlocal KR
        KR = carve_t([T], BF16)
        CN = carve_t([8 * T], BF16).rearrange("p (k n) -> p k n", k=8)
        CB = carve_t([4 * TC], F32).rearrange("p (k n) -> p k n", k=4)
        SQ = [carve_t([TC], F32) for _ in range(2)]
        RS = carve_t([TC], F32)
        TB = [carve_t([TC], F32) for _ in range(2)]
        T1 = carve_t([TC], F32)
        T2 = carve_t([TC], F32)
        wsrc = mla_w_down[j].rearrange("(k p) n -> p k n", p=128)
        r1keys = tuple(("R1", n) for n in range(NCH))

        def rms_group(wt, rk, cb0, goff, n, cs):
            for m in range(4):
                bk = m
                pe_acc(bank(bk), [(wt[:, k, (cb0 + m) * 128:(cb0 + m + 1) * 128], R1[:, k, cs]) for k in range(16)],
                       (rk, ("R1", n)), (("ps", bk),))
                act_copy(CB[:, m, :], bank(bk), (("ps", bk),), (("CB", m),))
                sq = SQ[m % 2]
                act(lambda e, sq=sq, bk=bk: e.activation(sq, bank(bk), AF.Square), (("ps", bk),), (("SQ", m % 2),))
                P.op("pe", lambda e, sq=sq, m=m: e.matmul(bank(4), lhsT=ones_f, rhs=sq, start=(m == 0), stop=(m == 3)),
                     (("SQ", m % 2), "cstf"), (("ps", 4),))
            dve(lambda e: e.tensor_scalar(RS, bank(4), 1.0 / 512.0, RMS_EPS, op0=ALU.mult, op1=ALU.add),
                (("ps", 4),), ("RS",))
            act(lambda e: e.activation(RS, RS, AF.Sqrt), ("RS",), ("RS",))
            dve(lambda e: e.reciprocal(RS, RS), ("RS",), ("RS",))
            for m in range(4):
                gcol = qkvn[:, j * 8 + goff + m: j * 8 + goff + m + 1]
                dve(lambda e, m=m, gcol=gcol: e.scalar_tensor_tensor(CN[:, goff + m, cs], CB[:, m, :], gcol, RS,
                                                                     op0=ALU.mult, op1=ALU.mult),
                    (("CB", m), "RS", "vecs"), (("CN", goff + m),))

        ri, rk = ring_next()
        wt = ring[ri][:, 0:16 * 512].rearrange("p (k n) -> p k n", k=16)
        wdma(wt, wsrc[:, :, 0:512], rk)
        for n in range(NCH):
            rms_group(wt, rk, 0, 0, n, slice(n * TC, (n + 1) * TC))
        ri, rk = ring_next()
        wt = ring[ri][:, 0:16 * 640].rearrange("p (k n) -> p k n", k=16)
        wdma(wt[:, :, 0:576], wsrc[:, :, 512:1088], rk)
        dve(lambda e, wt=wt: e.tensor_copy(wt[:, :, 576:608], wt[:, :, 544:576]), (rk,), (rk,))
        dve(lambda e, wt=wt: e.tensor_copy(wt[:, :, 608:640], wt[:, :, 512:544]), (rk,), (rk,))
        for n in range(NCH):
            cs = slice(n * TC, (n + 1) * TC)
            rms_group(wt, rk, 0, 4, n, cs)
            dma_sp(TB[0][0:64, :], tab[0, :, cs], (("tab", 0),), (("TB", 0),))
            dma_sp(TB[1][0:64, :], tab[1, :, cs], (("tab", 1),), (("TB", 1),))
            pe_acc(ps_all[0:64, 5 * 512:6 * 512], [(wt[:, k, 512:576], R1[:, k, cs]) for k in range(16)],
                   (rk, ("R1", n)), (("ps", 5),))
            pe_acc(ps_all[0:64, 6 * 512:7 * 512], [(wt[:, k, 576:640], R1[:, k, cs]) for k in range(16)],
                   (rk, ("R1", n)), (("ps", 6),))
            dve(lambda e: e.tensor_tensor(T1[0:64, :], ps_all[0:64, 5 * 512:6 * 512], TB[0][0:64, :], op=ALU.mult),
                (("ps", 5), ("TB", 0)), ("T1",))
            dve(lambda e: e.tensor_tensor(T2[0:64, :], ps_all[0:64, 6 * 512:7 * 512], TB[1][0:64, :], op=ALU.mult),
                (("ps", 6), ("TB", 1)), ("T2",))
            dve(lambda e, cs=cs: e.tensor_tensor(KR[0:64, cs], T1[0:64, :], T2[0:64, :], op=ALU.add),
                ("T1", "T2"), ("KR",))
        QST = [carve_t([T], BF16) for _ in range(2)]
        QRS = [carve_t([T], BF16) for _ in range(2)]
        usrc = mla_w_uq[j].rearrange("(k p) n -> p k n", p=128)
        for hg in range(2):
            ri, rk = ring_next()
            wt = ring[ri][:, 0:4 * 2048].rearrange("p (k n) -> p k n", k=4)
            wdma(wt[:, :, 0:1536], usrc[:, :, hg * 1536:(hg + 1) * 1536], rk)
            for hh in range(8):
                dve(lambda e, wt=wt, hh=hh: e.tensor_copy(wt[:, :, 1536 + hh * 64:1536 + hh * 64 + 32],
                                                          wt[:, :, hh * 192 + 160:hh * 192 + 192]), (rk,), (rk,))
                dve(lambda e, wt=wt, hh=hh: e.tensor_copy(wt[:, :, 1536 + hh * 64 + 32:1536 + hh * 64 + 64],
                                                          wt[:, :, hh * 192 + 128:hh * 192 + 160]), (rk,), (rk,))
            for hh in range(8):
                h = hg * 8 + hh
                hb = h % 2
                for n in range(NCH):
                    cs = slice(n * TC, (n + 1) * TC)
                    cnr = tuple(("CN", k) for k in range(4))
                    pe_acc(bank(0), [(wt[:, k, hh * 192:hh * 192 + 128], CN[:, k, cs]) for k in range(4)],
                           (rk,) + cnr, (("ps", 0),))
                    pe_acc(ps_all[0:64, 512:1024], [(wt[:, k, hh * 192 + 128:hh * 192 + 192], CN[:, k, cs]) for k in range(4)],
                           (rk,) + cnr, (("ps", 1),))
                    pe_acc(ps_all[0:64, 1024:1536], [(wt[:, k, 1536 + hh * 64:1536 + hh * 64 + 64], CN[:, k, cs]) for k in range(4)],
                           (rk,) + cnr, (("ps", 2),))
                    act_copy(QST[hb][:, cs], bank(0), (("ps", 0),), (("QST", hb),))
                    dma_sp(TB[0][0:64, :], tab[0, :, cs], (("tab", 0),), (("TB", 0),))
                    dma_sp(TB[1][0:64, :], tab[1, :, cs], (("tab", 1),), (("TB", 1),))
                    dve(lambda e: e.tensor_tensor(T1[0:64, :], ps_all[0:64, 512:1024], TB[0][0:64, :], op=ALU.mult),
                        (("ps", 1), ("TB", 0)), ("T1",))
                    dve(lambda e: e.tensor_tensor(T2[0:64, :], ps_all[0:64, 1024:1536], TB[1][0:64, :], op=ALU.mult),
                        (("ps", 2), ("TB", 1)), ("T2",))
                    dve(lambda e, cs=cs, hb=hb: e.tensor_tensor(QRS[hb][0:64, cs], T1[0:64, :], T2[0:64, :], op=ALU.add),
                        ("T1", "T2"), (("QRS", hb),))
                dma_sp(qn_d[h, :, :], QST[hb], (("QST", hb),), (("qn_d", h),), stream="sp_st")
                dma_sp(qr_d[h, :, :], QRS[hb][0:64, :], (("QRS", hb),), (("qr_d", h),), stream="sp_st")
        VST = [carve_t([TC], BF16) for _ in range(3)]
        ksrc = mla_w_ukv[j].rearrange("(k p) n -> p k n", p=128)
        cnt = 0
        for hg in range(2):
            ri, rk = ring_next()
            wt = ring[ri][:, 0:4 * 2048].rearrange("p (k n) -> p k n", k=4)
            wdma(wt, ksrc[:, :, hg * 2048:(hg + 1) * 2048], rk)
            cnr = tuple(("CN", 4 + k) for k in range(4))
            for hh in range(8):
                h = hg * 8 + hh
                hb = h % 2
                for n in range(NCH):
                    cs = slice(n * TC, (n + 1) * TC)
                    bk = cnt % 4
                    cnt += 1
                    pe_acc(bank(bk), [(wt[:, k, hh * 256:hh * 256 + 128], CN[:, 4 + k, cs]) for k in range(4)],
                           (rk,) + cnr, (("ps", bk),))
                    act_copy(QST[hb][:, cs], bank(bk), (("ps", bk),), (("QST", hb),))
                dma_sp(kn_d[h, :, :], QST[hb], (("QST", hb),), (("kn_d", h),), stream="sp_st")
            for tt in range(NT):
                ts_ = slice(tt * 128, (tt + 1) * 128)
                for g2 in range(2):
                    bk = 4 + cnt % 4
                    b3 = cnt % 3
                    cnt += 1
                    for hs in range(4):
                        hc = (g2 * 4 + hs) * 256 + 128
                        pe_acc(ps_all[:, bk * 512 + hs * 128: bk * 512 + (hs + 1) * 128],
                               [(CN[:, 4 + k, ts_], wt[:, k, hc:hc + 128]) for k in range(4)],
                               (rk,) + cnr, (("ps", bk),))
                    act_copy(VST[b3], bank(bk), (("ps", bk),), (("VST", b3),))
                    c0 = (hg * 8 + g2 * 4) * 128
                    dma_sp(v_d[ts_, c0:c0 + 512], VST[b3], (("VST", b3),), (("v_d", tt, c0 // 512),), stream="sp_st")
        P.barrier()
        aoff[0] = A_BASE + ((T * 2 + 63) // 64) * 64
        attention("mla")
        if j == 0:
            dump_r1("oT0", tuple(("R1o", h) for h in range(16)))
        P.barrier()
        phase_reset()
        out_proj(mla_w_o[j])
        P.barrier()

    KR = None

    def moba_mixer(j):
        phase_reset()
        if int(os.environ.get("K_STOP", "9")) <= 0:
            return
        QST = [carve_t([T], BF16) for _ in range(2)]
        VST = [carve_t([TC], BF16) for _ in range(3)]
        TB = [carve_t([TC], F32) for _ in range(2)]
        T1 = carve_t([TC], F32)
        T2 = carve_t([TC], F32)
        RO = carve_t([TC], BF16)
        wsrc = moba_w_qkv[j].rearrange("(k p) n -> p k n", p=128)
        r1keys = tuple(("R1", n) for n in range(NCH))
        cnt = 0
        for qk in range(2):
            dst = qn_d if qk == 0 else kn_d
            dkey = "qn_d" if qk == 0 else "kn_d"
            for hg in range(4):
                ri, rk = ring_next()
                wt = ring[ri][:, 0:16 * 768].rearrange("p (k n) -> p k n", k=16)
                c0 = qk * 2048 + hg * 512
                wdma(wt[:, :, 0:512], wsrc[:, :, c0:c0 + 512], rk)
                for hh in range(4):
                    wdma(wt[:, :, 512 + hh * 32:512 + hh * 32 + 16], wsrc[:, :, c0 + hh * 128 + 16:c0 + hh * 128 + 32], rk)
                    wdma(wt[:, :, 512 + hh * 32 + 16:512 + hh * 32 + 32], wsrc[:, :, c0 + hh * 128:c0 + hh * 128 + 16], rk)
                wdma(wt[:, :, 640:768], wsrc[:, :, c0:c0 + 128], rk)
                for hh in range(4):
                    h = hg * 4 + hh
                    hb = h % 2
                    for n in range(NCH):
                        cs = slice(n * TC, (n + 1) * TC)
                        bk = cnt % 2
                        cnt += 1
                        pe_acc(bank(bk), [(wt[:, k, hh * 128:(hh + 1) * 128], R1[:, k, cs]) for k in range(16)],
                               (rk, ("R1", n)), (("ps", bk),))
                        pe_acc(ps_all[:, (2 + bk) * 512:(3 + bk) * 512],
                               [(wt[:, k, 512 + hh * 32:512 + hh * 32 + 128], R1[:, k, cs]) for k in range(16)],
                               (rk, ("R1", n)), (("ps", 2 + bk),))
                        act_copy(QST[hb][:, cs], bank(bk), (("ps", bk),), (("QST", hb),))
                        if os.environ.get("K_NOROPE"):
                            continue
                        dma_sp(TB[0][0:32, :], tab[2, 0:32, cs], (("tab", 2),), (("TB", 0),))
                        dma_sp(TB[1][0:32, :], tab[3, 0:32, cs], (("tab", 3),), (("TB", 1),))
                        dve(lambda e, bk=bk: e.tensor_tensor(T1[0:32, :], ps_all[0:32, bk * 512:(bk + 1) * 512],
                                                             TB[0][0:32, :], op=ALU.mult), (("ps", bk), ("TB", 0), ("QST", hb)), ("T1",))
                        dve(lambda e, bk=bk: e.tensor_tensor(T2[0:32, :], ps_all[0:32, (2 + bk) * 512:(3 + bk) * 512],
                                                             TB[1][0:32, :], op=ALU.mult), (("ps", 2 + bk), ("TB", 1)), ("T2",))
                        dve(lambda e, cs=cs, hb=hb: e.tensor_tensor(QST[hb][0:32, cs], T1[0:32, :], T2[0:32, :], op=ALU.add),
                            ("T1", "T2", ("QST", hb)), (("QST", hb),))
                    dma_sp(dst[h, :, :], QST[hb], (("QST", hb),), ((dkey, h),), stream="sp_st")
        KS = int(os.environ.get("K_STOP", "9"))
        if KS <= 1:
            P.barrier()
            return
        for hg in range(4):
            ri, rk = ring_next()
            wt = ring[ri][:, 0:16 * 512].rearrange("p (k n) -> p k n", k=16)
            c0 = 4096 + hg * 512
            wdma(wt, wsrc[:, :, c0:c0 + 512], rk)
            for tt in range(NT):
                ts_ = slice(tt * 128, (tt + 1) * 128)
                bk = 4 + cnt % 4
                b3 = cnt % 3
                cnt += 1
                pe_acc(bank(bk), [(R1[:, k, ts_], wt[:, k, :]) for k in range(16)], (rk,) + r1keys, (("ps", bk),))
                act_copy(VST[b3], bank(bk), (("ps", bk),), (("VST", b3),))
                dma_sp(v_d[ts_, hg * 512:(hg + 1) * 512], VST[b3], (("VST", b3),), (("v_d", tt, hg),), stream="sp_st")
        P.barrier()
        if KS <= 2:
            return
        phase_reset()
        attention("moba")
        P.barrier()
        if KS <= 3:
            return
        phase_reset()
        out_proj(moba_w_o[j])
        P.barrier()

    def swiglu_pass(xs_fn, xs_keys, N, wgu, wdn, mode, H, out_fn):
        SG = [carve_t([CAP], F32) for _ in range(2)]
        gsrc = wgu.rearrange("(k p) n -> p k n", p=128)
        dsrc = wdn.rearrange("(k p) n -> p k n", p=128)
        chunks = [(0, min(512, N))] + ([(512, N - 512)] if N > 512 else [])
        cnt = 0
        for tg in range(22):
            ri, rk = ring_next()
            wt = ring[ri][:, 0:16 * 512].rearrange("p (k n) -> p k n", k=16)
            wdma(wt[:, :, 0:256], gsrc[:, :, tg * 256:(tg + 1) * 256], rk)
            wdma(wt[:, :, 256:512], gsrc[:, :, DFF + tg * 256:DFF + (tg + 1) * 256], rk)
            for b2 in range(2):
                jb = tg * 2 + b2
                a2 = cnt % 2
                cnt += 1
                gb, ub = 4 * a2, 4 * a2 + 2
                for (base, col0) in ((gb, b2 * 128), (ub, 256 + b2 * 128)):
                    def fn(e, base=base, col0=col0, wt=wt):
                        ins = None
                        for ci, (c0, w) in enumerate(chunks):
                            o = (base + ci) * 512
                            for k in range(16):
                                ins = e.matmul(ps_all[:, o:o + w], lhsT=wt[:, k, col0:col0 + 128],
                                               rhs=xs_fn(k)[:, c0:c0 + w], start=(k == 0), stop=(k == 15))
                        return ins
                    P.op("pe", fn, (rk,) + tuple(xs_keys), (("ps", base), ("ps", base + 1)))
                sg = SG[a2]
                for ci, (c0, w) in enumerate(chunks):
                    act(lambda e, sg=sg, gb=gb, ci=ci, c0=c0, w=w: e.activation(
                        sg[:, c0:c0 + w], ps_all[:, (gb + ci) * 512:(gb + ci) * 512 + w], AF.Silu),
                        (("ps", gb), ("ps", gb + 1)), (("SG", a2),))
                    dve(lambda e, sg=sg, ub=ub, ci=ci, c0=c0, w=w, jb=jb: e.tensor_tensor(
                        H[:, jb, c0:c0 + w], sg[:, c0:c0 + w], ps_all[:, (ub + ci) * 512:(ub + ci) * 512 + w], op=ALU.mult),
                        (("SG", a2), ("ps", ub), ("ps", ub + 1)), (("H", jb),))
        hkeys = tuple(("H", jb) for jb in range(NKF))
        cnt = 0
        for tw in range(8):
            ri, rk = ring_next()
            wt = ring[ri][:, 0:NKF * 256].rearrange("p (k n) -> p k n", k=NKF)
            wdma(wt[:, 0:22, :], dsrc[:, 0:22, tw * 256:(tw + 1) * 256], rk)
            wdma(wt[:, 22:44, :], dsrc[:, 22:44, tw * 256:(tw + 1) * 256], rk)
            if mode == "B":
                for b2 in range(2):
                    m = tw * 2 + b2
                    a2 = cnt % 4
                    cnt += 1
                    base = 2 * a2

                    def fn(e, base=base, b2=b2, wt=wt):
                        ins = None
                        for ci, (c0, w) in enumerate(chunks):
                            o = (base + ci) * 512
                            for k in range(NKF):
                                ins = e.matmul(ps_all[:, o:o + w], lhsT=wt[:, k, b2 * 128:(b2 + 1) * 128],
                                               rhs=H[:, k, c0:c0 + w], start=(k == 0), stop=(k == NKF - 1))
                        return ins
                    P.op("pe", fn, (rk,) + hkeys, (("ps", base), ("ps", base + 1)))
                    out_fn(m, [(ps_all[:, (base + ci) * 512:(base + ci) * 512 + w], c0, w, base + ci)
                               for ci, (c0, w) in enumerate(chunks)])
            else:
                for st in range(NST):
                    bk = cnt % 8
                    cnt += 1
                    pe_acc(ps_all[:, bk * 512:bk * 512 + 256],
                           [(H[:, k, st * 128:(st + 1) * 128], wt[:, k, :]) for k in range(NKF)],
                           (rk,) + hkeys, (("ps", bk),))
                    out_fn(tw, st, ps_all[:, bk * 512:bk * 512 + 256], bk)

    def dense_ffn(j):
        phase_reset()
        H = carve_t([NKF * CAP], BF16).rearrange("p (k n) -> p k n", k=NKF)
        YST = [carve_t([TC], F32) for _ in range(3)]
        ycnt = [0]
        for n in range(NCH):
            cs = slice(n * TC, (n + 1) * TC)

            def out_fn(m, lst, cs=cs, n=n):
                for (ap, c0, w, bk) in lst:
                    b3 = ycnt[0] % 3
                    ycnt[0] += 1
                    act_copy(YST[b3], ap, (("ps", bk),), (("YST", b3),))
                    dma_sp(yS[m, :, cs], YST[b3], (("YST", b3),), (("yS", n, m),), stream="sp_st")
            swiglu_pass(lambda k, cs=cs: R1[:, k, cs], (("R1", n),), TC, ffn_w_gate_up[j], ffn_w_down[j], "B", H, out_fn)
            aoff[0] -= 2 * ((CAP * 4 + 63) // 64 * 64)
        P.barrier()

    def moe_ffn(j):
        phase_reset()
        GW = RT[:, 0:128].rearrange("p (t e) -> p t e", e=8)
        SEL = RT[:, 128:256].rearrange("p (t e) -> p t e", e=8)
        POS = RT[:, 256:384].rearrange("p (t e) -> p t e", e=8)
        LG = RT[:, 384:392]
        M8 = RT[:, 392:400]
        EX = RT[:, 400:408]
        NG = RT[:, 408:409]
        DEN = RT[:, 409:410]
        SELB = RTB[:, :].rearrange("p (t e) -> p t e", e=8)
        WRv = WR[:, :].rearrange("p (k e) -> p k e", e=8)
        r1keys = tuple(("R1", n) for n in range(NCH))
        P.op("pool", lambda e: e.dma_start(out=WRv, in_=moe_w_router[j].rearrange("(k p) e -> p k e", p=128)),
             (), ("WR",), dma="pool_WR")
        for tt in range(NT):
            ts_ = slice(tt * 128, (tt + 1) * 128)
            bk = tt % 2
            pe_acc(ps_all[:, bk * 512:bk * 512 + 8], [(R1[:, k, ts_], WRv[:, k, :]) for k in range(16)],
                   ("WR",) + r1keys, (("ps", bk),))
            dve(lambda e, bk=bk: e.tensor_copy(LG, ps_all[:, bk * 512:bk * 512 + 8]), (("ps", bk),), ("LG",))
            dve(lambda e: e.max(M8, LG), ("LG",), ("M8",))
            dve(lambda e, tt=tt: e.tensor_scalar(SEL[:, tt, :], LG, M8[:, 1:2], None, op0=ALU.is_ge), ("LG", "M8"), ("SEL",))
            dve(lambda e: e.tensor_scalar(NG, M8[:, 0:1], -1.0, None, op0=ALU.mult), ("M8",), ("NG",))
            act(lambda e: e.activation(EX, LG, AF.Exp, bias=NG, scale=1.0), ("LG", "NG"), ("EX",))
            dve(lambda e, tt=tt: e.tensor_tensor(EX, EX, SEL[:, tt, :], op=ALU.mult), ("EX", "SEL"), ("EX",))
            dve(lambda e: e.reduce_sum(DEN, EX, axis=AX.X), ("EX",), ("DEN",))
            dve(lambda e: e.reciprocal(DEN, DEN), ("DEN",), ("DEN",))
            dve(lambda e, tt=tt: e.tensor_scalar(GW[:, tt, :], EX, DEN, None, op0=ALU.mult), ("EX", "DEN"), ("GW",))
            dve(lambda e, tt=tt: e.tensor_copy(SELB[:, tt, :], SEL[:, tt, :]), ("SEL",), ("SELB",))
        for tt in range(NT):
            bk = 2 + tt % 2
            pairs = [(ones_b, SELB[:, t2, :]) for t2 in range(tt)] + [(ltri, SELB[:, tt, :])]
            pe_acc(ps_all[:, bk * 512:bk * 512 + 8], pairs, ("SELB", "cstb"), (("ps", bk),))
            dve(lambda e, tt=tt, bk=bk: e.tensor_copy(POS[:, tt, :], ps_all[:, bk * 512:bk * 512 + 8]),
                (("ps", bk),), ("POS",))
        UST = [carve_t([2048], BF16) for _ in range(2)]
        for tt in range(NT):
            ts_ = slice(tt * 128, (tt + 1) * 128)
            ub = tt % 2
            for g in range(4):
                bk = 4 + (tt * 4 + g) % 4

                def fn(e, g=g, bk=bk, ts_=ts_):
                    ins = None
                    for q in range(4):
                        fb = g * 4 + q
                        ins = e.transpose(ps_bf[:, bk * 1024 + q * 128: bk * 1024 + (q + 1) * 128], R1[:, fb, ts_], ident_b)
                    return ins
                P.op("pe", fn, r1keys + ("cstb",), (("ps", bk),))
                act_copy(UST[ub][:, g * 512:(g + 1) * 512], ps_bf[:, bk * 1024: bk * 1024 + 512], (("ps", bk),), (("UST", ub),))
            dma_sp(ut_d[ts_, :], UST[ub], (("UST", ub),), (("ut_d", tt),), stream="sp_st")
        P.barrier()
        aoff[0] = 0
        H = carve_t([NKF * CAP], BF16).rearrange("p (k n) -> p k n", k=NKF)
        assert aoff[0] >= A_BASE
        XS = carve_t([NFB * CAP], BF16).rearrange("p (k n) -> p k n", k=NFB)
        PEB = carve_t([NT * CAP], BF16).rearrange("p (t n) -> p t n", t=NT)
        PW = [carve_t([CAP], BF16) for _ in range(2)]
        PTS = [carve_t([NST * 128], BF16).rearrange("p (s q) -> p s q", s=NST) for _ in range(2)]
        UTB = [carve_t([NT * 128], BF16).rearrange("p (t f) -> p t f", t=NT) for _ in range(3)]
        YSS = [carve_t([256], BF16) for _ in range(4)]
        cnt_t = 0
        cnt_u = 0
        ycnt = [0]
        for ex in range(NE):
            for tt in range(NT):
                ts_ = slice(tt * 128, (tt + 1) * 128)
                dve(lambda e, tt=tt, ex=ex: e.tensor_scalar(PEB[:, tt, :], iota_s, POS[:, tt, ex:ex + 1], SEL[:, tt, ex:ex + 1],
                                                            op0=ALU.is_equal, op1=ALU.mult),
                    ("POS", "SEL", "cstf"), (("PEB", tt),))
                pw = PW[tt % 2]
                dve(lambda e, tt=tt, ex=ex, pw=pw: e.tensor_scalar(pw, PEB[:, tt, :], GW[:, tt, ex:ex + 1], None, op0=ALU.mult),
                    (("PEB", tt), "GW"), (("PW", tt % 2),))
                bk = 6 + cnt_t % 2
                pts = PTS[cnt_t % 2]
                kpts = ("PTS", cnt_t % 2)
                cnt_t += 1

                def fn(e, pw=pw, bk=bk):
                    ins = None
                    for st in range(NST):
                        ins = e.transpose(ps_bf[:, bk * 1024 + st * 128: bk * 1024 + (st + 1) * 128],
                                          pw[:, st * 128:(st + 1) * 128], ident_b)
                    return ins
                P.op("pe", fn, (("PW", tt % 2), "cstb"), (("ps", bk),))
                act_copy(pts, ps_bf[:, bk * 1024: bk * 1024 + NST * 128].rearrange("p (s q) -> p s q", s=NST),
                         (("ps", bk),), (kpts,))
                dma_sp(ptw_d[ex, :, :, ts_].rearrange("s p q -> p s q"), pts, (kpts,), (("ptw_d", ex, tt),), stream="sp_st")
            pebkeys = tuple(("PEB", tt) for tt in range(NT))
            for fb in range(NFB):
                utb = UTB[cnt_u % 3]
                kut = ("UTB", cnt_u % 3)
                a2 = cnt_u % 2
                cnt_u += 1
                dma_sp(utb, ut_d[:, fb * 128:(fb + 1) * 128].rearrange("(t p) f -> p t f", p=128), tuple(("ut_d", t_) for t_ in range(NT)), (kut,))
                base = 2 * a2

                def fn(e, utb=utb, base=base):
                    ins = None
                    for ci, (c0, w) in enumerate(((0, 512), (512, CAP - 512))):
                        o = (base + ci) * 512
                        for tt in range(NT):
                            ins = e.matmul(ps_all[:, o:o + w], lhsT=utb[:, tt, :], rhs=PEB[:, tt, c0:c0 + w],
                                           start=(tt == 0), stop=(tt == NT - 1))
                    return ins
                P.op("pe", fn, (kut,) + pebkeys, (("ps", base), ("ps", base + 1)))
                act_copy(XS[:, fb, 0:512], ps_all[:, base * 512:(base + 1) * 512], (("ps", base),), (("XS", fb),))
                act_copy(XS[:, fb, 512:CAP], ps_all[:, (base + 1) * 512:(base + 1) * 512 + CAP - 512], (("ps", base + 1),), (("XS", fb),))

            def out_fn(tw, st, ap, bk, ex=ex):
                b4 = ycnt[0] % 4
                ycnt[0] += 1
                act_copy(YSS[b4], ap, (("ps", bk),), (("YSS", b4),))
                dma_sp(ys_d[ex, st * 128:(st + 1) * 128, tw * 256:(tw + 1) * 256], YSS[b4], (("YSS", b4),), (("ys_d", ex, st, tw),),
                       stream="sp_st")
            mark = aoff[0]
            swiglu_pass(lambda k: XS[:, k, :], tuple(("XS", fb) for fb in range(NFB)), CAP,
                        moe_w_gate_up[j, ex], moe_w_down[j, ex], "A", H, out_fn)
            aoff[0] = mark
        P.barrier()
        phase_reset()
        YSB = [carve_t([1024], BF16) for _ in range(4)]
        PTB = [carve_t([TC], BF16) for _ in range(4)]
        YST = [carve_t([TC], F32) for _ in range(3)]
        cnt = 0
        ycnt2 = 0
        for n in range(NCH):
            cs = slice(n * TC, (n + 1) * TC)
            for half in range(2):
                for ex in range(NE):
                    for st in range(NST):
                        b4 = cnt % 4
                        cnt += 1
                        dma_sp(YSB[b4], ys_d[ex, st * 128:(st + 1) * 128, half * 1024:(half + 1) * 1024], tuple(("ys_d", ex, st, half * 4 + t_) for t_ in range(4)), (("YSB", b4),))
                        dma_sp(PTB[b4], ptw_d[ex, st, :, cs], tuple(("ptw_d", ex, n * 4 + t_) for t_ in range(4)), (("PTB", b4),))
                        first = (ex == 0 and st == 0)
                        lastm = (ex == NE - 1 and st == NST - 1)

                        def fn(e, b4=b4, first=first, lastm=lastm):
                            ins = None
                            for f8 in range(8):
                                ins = e.matmul(bank(f8), lhsT=YSB[b4][:, f8 * 128:(f8 + 1) * 128], rhs=PTB[b4],
                                               start=first, stop=lastm)
                            return ins
                        P.op("pe", fn, (("YSB", b4), ("PTB", b4)), tuple(("ps", f8) for f8 in range(8)))
                for f8 in range(8):
                    b3 = ycnt2 % 3
                    ycnt2 += 1
                    act_copy(YST[b3], bank(f8), (("ps", f8),), (("YST", b3),))
                    dma_sp(yS[half * 8 + f8, :, cs], YST[b3], (("YST", b3),), (("yS", n, half * 8 + f8),), stream="sp_st")
        P.barrier()

    ln_phase(0, 0, True, False)
    s = 0
    for l in range(DEPTH):
        j = l // 2
        for sub in range(2):
            if s >= nsub:
                break
            if sub == 0:
                if l % 2 == 0:
                    mla_mixer(j)
                else:
                    moba_mixer(j)
            else:
                if l % 2 == 0:
                    dense_ffn(j)
                else:
                    moe_ffn(j)
            s += 1
            ln_phase(l, sub, False, s == nsub)
    P.barrier(engines=("sp",))

    with ExitStack() as es:
        P.emit(es)
    return nc


def _consts():
    cf = np.zeros((128, 1024), np.float32)
    cf[:, 0:128] = np.eye(128, dtype=np.float32)
    cf[:, 128:128 + CAP] = np.arange(CAP, dtype=np.float32)[None, :]
    om = np.zeros((8, 8), np.float32)
    for own in range(8):
        om[own, own:] = NEGB
    extra = np.zeros((128, 96), np.float32)
    extra[:, 0:64] = om.reshape(1, 64)
    p = np.arange(128)
    invm = np.zeros(128, np.float32)
    invm[:64] = (np.float32(THETA) ** (-(np.arange(0, 64, 2, dtype=np.float32)) / np.float32(64)))[p[:64] % 32]
    invb = np.zeros(128, np.float32)
    invb[:32] = (np.float32(THETA) ** (-(np.arange(0, 32, 2, dtype=np.float32)) / np.float32(32)))[p[:32] % 16]
    extra[:, 64] = invm
    extra[:, 65] = invb
    sg = np.ones(128, np.float32)
    sg[:32] = -1.0
    extra[:, 66] = sg
    sg2 = np.ones(128, np.float32)
    sg2[:16] = -1.0
    extra[:, 67] = sg2
    cf[:, 896:1024] = 1.0
    cb = np.zeros((128, 1024), np.float32)
    cb[:, 0:128] = np.eye(128)
    q = np.arange(128)[:, None]
    k = np.arange(128)[None, :]
    cb[:, 128:256] = np.where(k <= q, 0.0, NEGB)
    cb[:, 256:384] = (q < k).astype(np.float32)
    cb[:, 384:512] = 1.0
    return cf, cb.astype(ml_dtypes.bfloat16), extra


def _fm(v):
    v = np.asarray(v, np.float32)
    return np.ascontiguousarray(v.reshape(-1, 128).T)


_NC_CACHE = {}


def _prep_core(b, inputs, cf, cb, extra):
    x = np.asarray(inputs["x"][b], np.float32)
    xT = np.ascontiguousarray(x.T).reshape(NFB, 128, T)
    vecs = np.zeros((128, 512), np.float32)
    vecs[:, 0:384] = _fm(np.asarray(inputs["b_ada"], np.float32).reshape(-1))
    vecs[:, 384:400] = _fm(inputs["c"][b])
    qk = np.zeros((128, 16), np.float32)
    for j in range(2):
        qk[:, j * 8:j * 8 + 4] = _fm(inputs["mla_q_norm"][j])
        qk[:, j * 8 + 4:j * 8 + 8] = _fm(inputs["mla_kv_norm"][j])
    vecs[:, 400:416] = qk
    vecs[:, 416:512] = extra
    lnp = np.zeros((128, 256), np.float32)
    for l in range(DEPTH):
        for idx, nm in enumerate(("ln_mix_g", "ln_mix_b", "ln_ffn_g", "ln_ffn_b")):
            lnp[:, (l * 4 + idx) * 16:(l * 4 + idx + 1) * 16] = _fm(inputs[nm][l])
    return {
        "xT": xT, "pos": np.asarray(inputs["positions"][b], np.int32).reshape(1, T),
        "cstf": cf, "cstb": cb, "vecs": vecs, "lnp": lnp,
    }


W_NAMES = ("w_ada", "mla_w_down", "mla_w_uq", "mla_w_ukv", "mla_w_o", "ffn_w_gate_up", "ffn_w_down",
           "moba_w_qkv", "moba_w_o", "moe_w_router", "moe_w_gate_up", "moe_w_down")


def run(inputs, nsub=8, dbg=False, trace=False):
    key = (nsub, dbg)
    if key not in _NC_CACHE:
        _NC_CACHE[key] = build(nsub, dbg)
    nc = _NC_CACHE[key]
    cf, cb, extra = _consts()
    names = list(W_NAMES)
    if nsub <= 1:
        names = [n for n in names if not n.startswith("ffn")]
    if nsub <= 2:
        names = [n for n in names if not n.startswith("mob")]
    if nsub <= 3:
        names = [n for n in names if not n.startswith("moe")]
    ws = {n: np.ascontiguousarray(np.asarray(inputs[n], np.float32)) for n in names}
    in_maps = []
    for b in range(NCORES):
        m = _prep_core(b, inputs, cf, cb, extra)
        m.update(ws)
        in_maps.append(m)
    res = run_bass_kernel_spmd(nc, in_maps, core_ids=list(range(NCORES)), trace=trace)
    return res


def kernel(**inputs):
    res = run(inputs)
    out = np.empty((NCORES, T, D), np.float32)
    for b in range(NCORES):
        oT = np.asarray(res.results[b]["outT"]).reshape(D, T)
        out[b] = oT.T
    return out
```

```python
import math
import os
import re
from contextlib import ExitStack
import numpy as np
import ml_dtypes
import concourse.bass as bass
import concourse.mybir as mybir
from concourse.bass_utils import run_bass_kernel_spmd

F32 = mybir.dt.float32
BF16 = mybir.dt.bfloat16
I32 = mybir.dt.int32
U8 = mybir.dt.uint8
ALU = mybir.AluOpType
AF = mybir.ActivationFunctionType
AX = mybir.AxisListType

T = 2048
D = 2048
NFB = 16
TC = 512
NCH = 4
NT = 16
DEPTH = 4
DFF = 5632
NKF = 44
NE = 8
CAP = 768
NST = 6
ALPHA = (2.0 * DEPTH) ** 0.25
LN_EPS = 1e-5
RMS_EPS = 1e-6
THETA = 500000.0
NEGB = -30000.0
NCORES = 4
RING_ELEMS = 12288
SEM_LIMIT = 28000

ENGS = ("sp", "act", "dve", "pool", "pe")


class Prog:
    def __init__(self, nc):
        self.nc = nc
        self.ops = {e: [] for e in ENGS}
        self.epoch = {}
        self.cnt = {}
        self.seen = {e: {} for e in ENGS}
        self.lastw = {}
        self.readers = {}
        self.ring_i = 0
        self.isdma = {}

    def _bump(self, stream, inc):
        ep = self.epoch.get(stream, 0)
        c = self.cnt.get((stream, ep), 0)
        if c + inc > SEM_LIMIT:
            ep += 1
            self.epoch[stream] = ep
            c = 0
        c += inc
        self.cnt[(stream, ep)] = c
        return (stream, ep, c)

    def op(self, eng, fn, reads=(), writes=(), dma=None):
        stream = dma if dma else eng
        self.isdma[stream] = bool(dma)
        waits = {}

        def need(tok):
            s, ep, v = tok
            if s == eng and not dma and eng == "pe":
                return
            if dma and s == stream:
                return
            k = (s, ep)
            if self.seen[eng].get(k, 0) >= v:
                return
            if waits.get(k, 0) < v:
                waits[k] = v

        for key in reads:
            lw = self.lastw.get(key)
            if lw is not None:
                need(lw)
        for key in writes:
            lw = self.lastw.get(key)
            if lw is not None:
                need(lw)
            for tok in self.readers.get(key, {}).values():
                need(tok)
        for k, v in waits.items():
            self.seen[eng][k] = v
        tok = self._bump(stream, 16 if dma else 1)
        for key in writes:
            self.lastw[key] = tok
            self.readers[key] = {}
        for key in reads:
            self.readers.setdefault(key, {})[(tok[0], tok[1])] = tok
        self.ops[eng].append((list(waits.items()), fn, (stream, tok[1]), 16 if dma else 1))
        return tok

    def barrier(self, engines=("sp", "act", "dve", "pe")):
        toks = []
        for (s, ep), c in self.cnt.items():
            toks.append((s, ep, c))
        for eng in engines:
            waits = {}
            for (s, ep, v) in toks:
                if s == eng:
                    continue
                if s.startswith("pool"):
                    continue
                k = (s, ep)
                if self.seen[eng].get(k, 0) >= v:
                    continue
                waits[k] = v
                self.seen[eng][k] = v
            if waits:
                self.ops[eng].append((list(waits.items()), None, None, 0))

    def emit(self, es):
        nc = self.nc
        sems = {}
        for (s, ep) in self.cnt.keys():
            sems[(s, ep)] = es.enter_context(nc.semaphore("s_%s_%d" % (s, ep)))
        block = es.enter_context(nc.Block())
        ops = self.ops

        def run(e, lst):
            for waits, fn, sig, inc in lst:
                for k, v in waits:
                    e.wait_ge(sems[k], v)
                if fn is None:
                    continue
                ins = fn(e)
                ins.then_inc(sems[sig], inc)

        @block.sync
        def _(e):
            run(e, ops["sp"])

        @block.scalar
        def _(e):
            run(e, ops["act"])

        @block.vector
        def _(e):
            run(e, ops["dve"])

        @block.gpsimd
        def _(e):
            run(e, ops["pool"])

        @block.tensor
        def _(e):
            run(e, ops["pe"])


def build(nsub=8, dbg=False):
    nc = bass.Bass("TRN2", target_bir_lowering=False)
    P = Prog(nc)

    def din(name, shape, dt=F32):
        return nc.dram_tensor(name, list(shape), dt, kind="ExternalInput")

    def dscr(name, shape, dt=F32):
        return nc.dram_tensor(name, list(shape), dt, kind=("ExternalOutput" if dbg else "Internal"))

    nlayers = (nsub + 1) // 2
    xT_in = din("xT", [NFB, 128, T])
    pos_in = din("pos", [1, T], I32)
    cstf_in = din("cstf", [128, 1024])
    cstb_in = din("cstb", [128, 1024], BF16)
    vecs_in = din("vecs", [128, 512])
    w_ada = din("w_ada", [DEPTH, D, 6 * D])
    mla_w_down = din("mla_w_down", [2, D, 1088])
    mla_w_uq = din("mla_w_uq", [2, 512, 3072])
    mla_w_ukv = din("mla_w_ukv", [2, 512, 4096])
    mla_w_o = din("mla_w_o", [2, D, D])
    if nsub > 1:
        ffn_w_gate_up = din("ffn_w_gate_up", [2, D, 2 * DFF])
        ffn_w_down = din("ffn_w_down", [2, DFF, D])
    if nsub > 2:
        moba_w_qkv = din("moba_w_qkv", [2, D, 6144])
        moba_w_o = din("moba_w_o", [2, D, D])
    if nsub > 3:
        moe_w_router = din("moe_w_router", [2, D, NE])
        moe_w_gate_up = din("moe_w_gate_up", [2, NE, D, 2 * DFF])
        moe_w_down = din("moe_w_down", [2, NE, DFF, D])
    outT = nc.dram_tensor("outT", [NFB, 128, T], F32, kind="ExternalOutput")

    xS = dscr("xS", [NFB, 128, T])
    yS = dscr("yS", [NFB, 128, T])
    tab = dscr("tab", [4, 64, T])
    qn_d = dscr("qn_d", [16, 128, T], BF16)
    kn_d = dscr("kn_d", [16, 128, T], BF16)
    qr_d = dscr("qr_d", [16, 64, T], BF16)
    v_d = dscr("v_d", [T, 2048], BF16)
    ut_d = dscr("ut_d", [T, 2048], BF16)
    ys_d = dscr("ys_d", [NE, CAP, 2048], BF16)
    ptw_d = dscr("ptw_d", [NE, NST, 128, T], BF16)

    ring = [nc.alloc_sbuf_tensor("ring%d" % i, [128, RING_ELEMS], BF16) for i in range(2)]
    cstf = nc.alloc_sbuf_tensor("cstf_sb", [128, 1024], F32)
    cstb = nc.alloc_sbuf_tensor("cstb_sb", [128, 1024], BF16)
    vecs = nc.alloc_sbuf_tensor("vecs_sb", [128, 512], F32)
    ADA = nc.alloc_sbuf_tensor("ADA", [128, 4 * 96], F32)
    PV = nc.alloc_sbuf_tensor("PVEC", [128, 64], F32)
    cact = nc.alloc_sbuf_tensor("cact", [128, 16], BF16)
    SM = nc.alloc_sbuf_tensor("SM", [128, 64], F32)
    RT = nc.alloc_sbuf_tensor("RT", [128, 16 * 8 * 4], F32)
    RTB = nc.alloc_sbuf_tensor("RTB", [128, 16 * 8], BF16)
    WR = nc.alloc_sbuf_tensor("WR", [128, 16 * 8], BF16)
    LNP = nc.alloc_sbuf_tensor("LNP", [128, 256], F32)
    remaining = nc.sbuf_bytes_remaining
    ARENA_BYTES = (remaining // 64) * 64 - 256
    arena = nc.alloc_sbuf_tensor("arena", [128, ARENA_BYTES], U8)
    ps_all = nc.alloc_psum_tensor("ps_all", [128, 4096], F32)
    ps_bf = ps_all[:, :].bitcast(BF16)

    ident_f = cstf[:, 0:128]
    iota_s = cstf[:, 128:128 + CAP]
    ownmask = vecs[:, 416:480]
    invf_mla = vecs[:, 480:481]
    invf_moba = vecs[:, 481:482]
    sgn_mla = vecs[:, 482:483]
    sgn_moba = vecs[:, 483:484]
    ones_f = cstf[:, 896:1024]
    ident_b = cstb[:, 0:128]
    trimask = cstb[:, 128:256]
    ltri = cstb[:, 256:384]
    ones_b = cstb[:, 384:512]
    V_BADA = 0
    V_LNP = 384

    aoff = [0]

    def carve(nbytes):
        o = aoff[0]
        nb = (nbytes + 63) // 64 * 64
        assert o + nb <= ARENA_BYTES, ("arena overflow", o, nb, ARENA_BYTES)
        aoff[0] = o + nb
        return arena[:, o:o + nbytes]

    def carve_t(shape_free, dt):
        n = int(np.prod(shape_free))
        esz = 4 if dt == F32 or dt == I32 else 2
        v = carve(n * esz).bitcast(dt)
        return v

    def arena_reset():
        aoff[0] = 0

    def bank(i):
        return ps_all[:, i * 512:(i + 1) * 512]

    def _sname(key):
        return "d_" + re.sub(r"[^A-Za-z0-9]", "_", str(key))

    def dma_sp(out, in_, reads, writes, stream="sp_ld"):
        key = reads[0] if stream == "sp_st" else writes[0]
        P.op("sp", lambda e: e.dma_start(out=out, in_=in_), reads, writes, dma=_sname(key))

    def wdma(out, in_, rkey):
        P.op("pool", lambda e: e.dma_start(out=out, in_=in_), (), (rkey,), dma="pool_" + _sname(rkey))

    def ring_next():
        i = P.ring_i
        P.ring_i = 1 - i
        return i, ("ring", i)

    def pe_acc(out_ap, pairs, reads, writes):
        def fn(e):
            n = len(pairs)
            ins = None
            for i, (l, r) in enumerate(pairs):
                ins = e.matmul(out_ap, lhsT=l, rhs=r, start=(i == 0), stop=(i == n - 1))
            return ins
        P.op("pe", fn, reads, writes)

    def act_copy(out, in_, reads, writes):
        P.op("act", lambda e: e.copy(out, in_), reads, writes)

    def dve(fn, reads, writes):
        P.op("dve", fn, reads, writes)

    def act(fn, reads, writes):
        P.op("act", fn, reads, writes)

    dma_sp(cstf[:, :], cstf_in[:, :], (), ("cstf",))
    dma_sp(cstb[:, :], cstb_in[:, :], (), ("cstb",))
    dma_sp(vecs[:, :], vecs_in[:, :], (), ("vecs",))
    lnp_in = din("lnp", [128, 256])
    dma_sp(LNP[:, :], lnp_in[:, :], (), ("lnp",))
    cT = vecs[:, 384:400]
    qkvn = vecs[:, 400:416]
    act(lambda e: e.activation(cact[:, :], cT, AF.Silu), ("vecs",), ("cact",))

    ADA_BASE = 7 * 512 + 16

    def ada_tile(l, jt):
        ri, rk = ring_next()
        wt = ring[ri][:, 0:16 * 512].rearrange("p (k n) -> p k n", k=16)
        src = w_ada[l].rearrange("(k p) n -> p k n", p=128)
        wdma(wt, src[:, :, jt * 512:(jt + 1) * 512], rk)

        def fn(e):
            ins = None
            for blk in range(4):
                col = ADA_BASE + jt * 4 + blk
                for k in range(16):
                    ins = e.matmul(ps_all[:, col:col + 1], lhsT=wt[:, k, blk * 128:(blk + 1) * 128],
                                   rhs=cact[:, k:k + 1], start=(k == 0), stop=(k == 15))
            return ins
        P.op("pe", fn, (rk, "cact"), (("ps", 7),))

    def ada_finish(l):
        dve(lambda e: e.tensor_tensor(ADA[:, l * 96:(l + 1) * 96], ps_all[:, ADA_BASE:ADA_BASE + 96],
                                      vecs[:, l * 96:(l + 1) * 96], op=ALU.add),
            (("ps", 7), "vecs"), ("ADA",))

    for jt in range(24):
        ada_tile(0, jt)
    ada_finish(0)

    def ada(l, idx):
        return ADA[:, l * 96 + idx * 16: l * 96 + (idx + 1) * 16]

    def lnp(l, idx):
        return LNP[:, (l * 4 + idx) * 16:(l * 4 + idx + 1) * 16]

    arena_reset()
    posi = carve_t([T], I32)
    posf = carve_t([T], F32)
    ang = carve_t([T], F32)
    tmpa = carve_t([T], F32)
    tmps = carve_t([T], F32)
    dma_sp(posi[0:64, :], pos_in[0, :].partition_broadcast(64), (), ("posi",))
    dve(lambda e: e.tensor_copy(posf[0:64, :], posi[0:64, :]), ("posi",), ("posf",))
    tmpi = carve_t([T], I32)
    TWO_PI = 2.0 * math.pi

    def range_reduce(npart, shift):
        dve(lambda e: e.tensor_scalar(tmpa[0:npart, :], ang[0:npart, :], shift, 1.0 / TWO_PI, op0=ALU.add, op1=ALU.mult),
            ("ang", "tmps"), ("tmpa",))
        dve(lambda e: e.tensor_copy(tmpi[0:npart, :], tmpa[0:npart, :]), ("tmpa",), ("tmpi",))
        dve(lambda e: e.tensor_copy(tmpa[0:npart, :], tmpi[0:npart, :]), ("tmpi",), ("tmpa",))
        dve(lambda e: e.scalar_tensor_tensor(tmpa[0:npart, :], tmpa[0:npart, :], -TWO_PI, ang[0:npart, :],
                                             op0=ALU.mult, op1=ALU.add), ("tmpa", "ang"), ("tmpa",))
        dve(lambda e: e.tensor_scalar_add(tmpa[0:npart, :], tmpa[0:npart, :], shift), ("tmpa",), ("tmpa",))
        dve(lambda e: e.tensor_scalar(tmps[0:npart, :], tmpa[0:npart, :], math.pi, TWO_PI, op0=ALU.is_gt, op1=ALU.mult),
            ("tmpa",), ("tmps",))
        dve(lambda e: e.tensor_tensor(tmpa[0:npart, :], tmpa[0:npart, :], tmps[0:npart, :], op=ALU.subtract),
            ("tmpa", "tmps"), ("tmpa",))
        dve(lambda e: e.tensor_scalar(tmps[0:npart, :], tmpa[0:npart, :], -math.pi, TWO_PI, op0=ALU.is_lt, op1=ALU.mult),
            ("tmpa",), ("tmps",))
        dve(lambda e: e.tensor_tensor(tmpa[0:npart, :], tmpa[0:npart, :], tmps[0:npart, :], op=ALU.add),
            ("tmpa", "tmps"), ("tmpa",))
        dve(lambda e: e.tensor_scalar(tmpa[0:npart, :], tmpa[0:npart, :], math.pi, -math.pi, op0=ALU.min, op1=ALU.max),
            ("tmpa",), ("tmpa",))

    for ti, (npart, invf, sgn) in enumerate(((64, invf_mla, sgn_mla), (32, invf_moba, sgn_moba))):
        dve(lambda e, npart=npart, invf=invf: e.tensor_scalar(ang[0:npart, :], posf[0:npart, :], invf[0:npart, :], None,
                                                              op0=ALU.mult), ("posf", "vecs", "tmpa"), ("ang",))
        range_reduce(npart, 0.5 * math.pi)
        act(lambda e, npart=npart: e.activation(tmps[0:npart, :], tmpa[0:npart, :], AF.Sin), ("tmpa",), ("tmps",))
        dma_sp(tab[2 * ti, 0:npart, :], tmps[0:npart, :], ("tmps",), (("tab", 2 * ti),), stream="sp_st")
        range_reduce(npart, 0.0)
        act(lambda e, npart=npart: e.activation(tmps[0:npart, :], tmpa[0:npart, :], AF.Sin), ("tmpa",), ("tmps",))
        dve(lambda e, npart=npart, sgn=sgn: e.tensor_scalar(tmps[0:npart, :], tmps[0:npart, :], sgn[0:npart, :], None,
                                                            op0=ALU.mult), ("tmps", "vecs"), ("tmps",))
        dma_sp(tab[2 * ti + 1, 0:npart, :], tmps[0:npart, :], ("tmps",), (("tab", 2 * ti + 1),), stream="sp_st")
    P.barrier()

    arena_reset()
    R1 = carve_t([NFB, T], BF16).rearrange("p (k n) -> p k n", k=NFB)
    A_BASE = aoff[0]

    def phase_reset():
        aoff[0] = A_BASE

    dump_i = [0]

    def dump_r1(name, keys):
        if not dbg:
            return
        dt_ = nc.dram_tensor("dbg_" + name, [128, NFB * T], BF16, kind="ExternalOutput")
        P.op("sp", lambda e: e.dma_start(out=dt_[:, :], in_=R1[:, :, :].rearrange("p k n -> p (k n)")), keys, (), dma="d_dump%d" % dump_i[0])
        dump_i[0] += 1

    def ln_phase(l, sub, first, last):
        phase_reset()
        XI = [carve_t([TC], F32) for _ in range(3)]
        if first:
            sc, sh = ada(0, 1), ada(0, 0)
            dve(lambda e: e.tensor_scalar_add(PV[:, 0:16], sc, 1.0), ("ADA",), ("PV",))
            for n in range(NCH):
                for fb in range(NFB):
                    xi = XI[(n * NFB + fb) % 3]
                    kx = ("XI", (n * NFB + fb) % 3)
                    dma_sp(xi, xT_in[fb, :, n * TC:(n + 1) * TC], (), (kx,))
                    dve(lambda e, xi=xi, fb=fb, n=n: e.tensor_scalar(
                        R1[:, fb, n * TC:(n + 1) * TC], xi, PV[:, fb:fb + 1], sh[:, fb:fb + 1],
                        op0=ALU.mult, op1=ALU.add), (kx, "PV", "ADA"), (("R1", n),))
            dump_r1("u0", tuple(("R1", n) for n in range(NCH)))
            P.barrier()
            return
        xsrc = xT_in if (l == 0 and sub == 0) else xS
        xdst = outT if last else xS
        g = ada(l, 2 + 3 * sub)
        gam, bet = lnp(l, 2 * sub), lnp(l, 2 * sub + 1)
        dve(lambda e: e.tensor_scalar_add(PV[:, 0:16], g, 1.0), ("ADA",), ("PV",))
        if not last:
            if sub == 0:
                sc, sh = ada(l, 4), ada(l, 3)
            else:
                sc, sh = ada(l + 1, 1), ada(l + 1, 0)
            dve(lambda e: e.tensor_scalar_add(PV[:, 48:64], sc, 1.0), ("ADA", "PV"), ("PV",))
            dve(lambda e: e.tensor_tensor(PV[:, 16:32], gam, PV[:, 48:64], op=ALU.mult), ("lnp", "PV"), ("PV",))
            dve(lambda e: e.tensor_tensor(PV[:, 32:48], bet, PV[:, 48:64], op=ALU.mult), ("lnp", "PV"), ("PV",))
            dve(lambda e: e.tensor_tensor(PV[:, 32:48], PV[:, 32:48], sh, op=ALU.add), ("PV", "ADA"), ("PV",))
        YI = [carve_t([TC], F32) for _ in range(3)]
        ZB = carve_t([NFB, TC], F32).rearrange("p (k n) -> p k n", k=NFB)
        SQ = [carve_t([TC], F32) for _ in range(2)]
        MB = carve_t([TC], F32)
        RB = carve_t([TC], F32)
        TMP = carve_t([TC], F32)
        XO = [carve_t([TC], F32) for _ in range(3)]
        cnt = 0
        for n in range(NCH):
            cs = slice(n * TC, (n + 1) * TC)
            for fb in range(NFB):
                b3 = cnt % 3
                b2 = cnt % 2
                cnt += 1
                xi, yi, sq = XI[b3], YI[b3], SQ[b2]
                kx, ky, ksq = ("XI", b3), ("YI", b3), ("SQ", b2)
                dma_sp(xi, xsrc[fb, :, cs], (("xS", n, fb),), (kx,))
                dma_sp(yi, yS[fb, :, cs], (("yS", n, fb),), (ky,))
                dve(lambda e, yi=yi, fb=fb: e.tensor_scalar(yi, yi, PV[:, fb:fb + 1], None, op0=ALU.mult),
                    (ky, "PV"), (ky,))
                dve(lambda e, xi=xi, yi=yi, fb=fb: e.scalar_tensor_tensor(ZB[:, fb, :], xi, ALPHA, yi,
                                                                          op0=ALU.mult, op1=ALU.add),
                    (kx, ky), (("ZB", fb),))
                act(lambda e, sq=sq, fb=fb: e.activation(sq, ZB[:, fb, :], AF.Square), (("ZB", fb),), (ksq,))
                P.op("pe", lambda e, fb=fb: e.matmul(bank(0), lhsT=ones_f, rhs=ZB[:, fb, :], start=(fb == 0),
                                                     stop=(fb == NFB - 1)), (("ZB", fb), "cstf"), (("ps", 0),))
                P.op("pe", lambda e, fb=fb, sq=sq: e.matmul(bank(1), lhsT=ones_f, rhs=sq, start=(fb == 0),
                                                            stop=(fb == NFB - 1)), (ksq, "cstf"), (("ps", 1),))
            dve(lambda e: e.tensor_scalar(MB, bank(0), 1.0 / D, None, op0=ALU.mult), (("ps", 0),), ("MB",))
            dve(lambda e: e.tensor_tensor(TMP, MB, MB, op=ALU.mult), ("MB",), ("TMP",))
            dve(lambda e: e.scalar_tensor_tensor(RB, bank(1), 1.0 / D, TMP, op0=ALU.mult, op1=ALU.subtract),
                (("ps", 1), "TMP"), ("RB",))
            dve(lambda e: e.tensor_scalar_add(RB, RB, LN_EPS), ("RB",), ("RB",))
            act(lambda e: e.activation(RB, RB, AF.Sqrt), ("RB",), ("RB",))
            dve(lambda e: e.reciprocal(RB, RB), ("RB",), ("RB",))
            for fb in range(NFB):
                b3 = fb % 3
                xo = XO[b3]
                kxo = ("XO", b3)
                dve(lambda e, fb=fb: e.tensor_tensor(ZB[:, fb, :], ZB[:, fb, :], MB, op=ALU.subtract),
                    (("ZB", fb), "MB"), (("ZB", fb),))
                dve(lambda e, fb=fb: e.tensor_tensor(ZB[:, fb, :], ZB[:, fb, :], RB, op=ALU.mult),
                    (("ZB", fb), "RB"), (("ZB", fb),))
                act(lambda e, fb=fb, xo=xo: e.activation(xo, ZB[:, fb, :], AF.Identity, bias=bet[:, fb:fb + 1],
                                                         scale=gam[:, fb:fb + 1]), (("ZB", fb), "lnp"), (kxo,))
                dma_sp(xdst[fb, :, cs], xo, (kxo,), (("xS", n, fb),), stream="sp_st")
                if not last:
                    act(lambda e, fb=fb, cs=cs: e.activation(R1[:, fb, cs], ZB[:, fb, :], AF.Identity,
                                                             bias=PV[:, 32 + fb:33 + fb], scale=PV[:, 16 + fb:17 + fb]),
                        (("ZB", fb), "PV"), (("R1", n),))
        P.barrier()

    def attention(kind, nheads=16, ada_l=None):
        scale = (192.0 ** -0.5) if kind == "mla" else (128.0 ** -0.5)
        QN = [carve_t([T], BF16) for _ in range(2)]
        KN = [carve_t([T], BF16) for _ in range(2)]
        QR = [carve_t([T], BF16) for _ in range(2)] if kind == "mla" else None
        VH = [carve_t([16 * 128], BF16).rearrange("p (t d) -> p t d", t=16) for _ in range(2)]
        PB = [carve_t([T], BF16) for _ in range(2)]
        PT = [carve_t([16 * 128], BF16).rearrange("p (t d) -> p t d", t=16) for _ in range(2)]
        DG = [carve_t([128], BF16) for _ in range(2)]
        KM = [carve_t([8], BF16) for _ in range(2)]
        KM32 = carve_t([8], F32)
        nogate = bool(os.environ.get("K_NOGATE"))
        kr_b = KR if kind == "mla" else None

        def loads(h):
            hb = h % 2
            dma_sp(QN[hb], qn_d[h, :, :], (("qn_d", h),), (("QN", hb),))
            dma_sp(KN[hb], kn_d[h, :, :], (("kn_d", h),), (("KN", hb),))
            dma_sp(VH[hb], v_d[:, h * 128:(h + 1) * 128].rearrange("(t p) d -> p t d", p=128),
                   tuple(("v_d", t_, h // 4) for t_ in range(NT)), (("VH", hb),))
            if kind == "mla":
                dma_sp(QR[hb][0:64, :], qr_d[h, :, :], (("qr_d", h),), (("QR", hb),))
            elif not nogate:
                kn = KN[hb]
                dve(lambda e, kn=kn: e.tensor_reduce(KM32, kn.rearrange("p (b k) -> p b k", b=8), axis=AX.X, op=ALU.add),
                    (("KN", hb),), ("KM32",))
                dve(lambda e, hb=hb: e.tensor_scalar(KM[hb], KM32, 1.0 / 256.0, None, op0=ALU.mult), ("KM32",), (("KM", hb),))

        def geom(t):
            h, i = divmod(t, NT)
            nk = (i + 1) * 128
            nchk = (nk + 511) // 512
            own = i // 2
            gated = (kind == "moba" and own >= 4 and not nogate)
            return h, i, nk, nchk, own, gated

        def emit_S(t):
            h, i, nk, nchk, own, gated = geom(t)
            hb = h % 2
            qs = slice(i * 128, (i + 1) * 128)
            qn, kn = QN[hb], KN[hb]
            qr_b = QR[hb] if kind == "mla" else None
            rd = [("QN", hb), ("KN", hb), "cstb"]
            if kind == "mla":
                rd += [("QR", hb), "KR"]

            def s_fn(e):
                ins = None
                for c in range(nchk):
                    w = min(512, nk - c * 512)
                    ks = slice(c * 512, c * 512 + w)
                    lastc = (c == nchk - 1)
                    e.matmul(ps_all[:, ks], lhsT=qn[:, qs], rhs=kn[:, ks], start=True,
                             stop=(kind != "mla" and not lastc))
                    if kind == "mla":
                        ins = e.matmul(ps_all[:, ks], lhsT=qr_b[0:64, qs], rhs=kr_b[0:64, ks], start=False,
                                       stop=(not lastc))
                    if lastc:
                        ins = e.matmul(ps_all[:, i * 128:(i + 1) * 128], lhsT=ident_b, rhs=trimask,
                                       start=False, stop=True)
                return ins
            P.op("pe", s_fn, rd, [("ps", c) for c in range(nchk)])
            if gated:
                P.op("pe", lambda e: e.matmul(ps_all[:, 7 * 512:7 * 512 + 8], lhsT=qn[:, qs], rhs=KM[hb],
                                              start=True, stop=True), (("QN", hb), ("KM", hb)), (("ps", 7),))

        def emit_softmax(t):
            h, i, nk, nchk, own, gated = geom(t)
            b2 = t % 2
            pb, dg = PB[b2], DG[b2]
            kpb, kdg = ("PB", b2), ("DG", b2)
            if gated:
                dve(lambda e: e.tensor_tensor(SM[:, 16:24], ps_all[:, 7 * 512:7 * 512 + 8],
                                              ownmask[:, own * 8:(own + 1) * 8], op=ALU.add),
                    (("ps", 7), "vecs"), ("SMg",))
                dve(lambda e: e.max(SM[:, 24:32], SM[:, 16:24]), ("SMg",), ("SMg",))
                dve(lambda e: e.tensor_scalar(SM[:, 32:40], SM[:, 16:24], SM[:, 26:27], None, op0=ALU.is_ge),
                    ("SMg",), ("SMg",))
                dve(lambda e: e.tensor_scalar(SM[:, 32:40], SM[:, 32:40], -1.0, -NEGB, op0=ALU.add, op1=ALU.mult),
                    ("SMg",), ("SMg",))
            pskeys = tuple(("ps", c) for c in range(nchk))
            dve(lambda e: e.reduce_max(SM[:, 4:5], ps_all[:, 0:nk], axis=AX.X), pskeys, ("SMm",))
            dve(lambda e: e.tensor_scalar(SM[:, 5:6], SM[:, 4:5], -scale, None, op0=ALU.mult), ("SMm",), ("SMm",))
            if gated:
                dve(lambda e: e.tensor_scalar(SM[:, 40:48], SM[:, 32:40], SM[:, 5:6], None, op0=ALU.add),
                    ("SMm", "SMg"), ("SMb",))
                nblk = own + 1
                for n in range(nblk):
                    k0 = n * 256
                    w = min(256, nk - k0)
                    bias_ap = SM[:, 40 + n:41 + n] if n < own else SM[:, 5:6]
                    act(lambda e, k0=k0, w=w, bias_ap=bias_ap, n=n: e.activation(
                        pb[:, k0:k0 + w], ps_all[:, k0:k0 + w], AF.Exp, bias=bias_ap, scale=scale,
                        accum_out=SM[:, 48 + n:49 + n]),
                        (("ps", k0 // 512), "SMb", "SMm"), (kpb, "SMs"))
                dve(lambda e: e.reduce_sum(SM[:, 6:7], SM[:, 48:48 + nblk], axis=AX.X), ("SMs",), ("SMr",))
            else:
                act(lambda e: e.activation(pb[:, 0:nk], ps_all[:, 0:nk], AF.Exp, bias=SM[:, 5:6],
                                           scale=scale, accum_out=SM[:, 6:7]),
                    pskeys + ("SMm",), (kpb, "SMr"))
            dve(lambda e: e.reciprocal(SM[:, 7:8], SM[:, 6:7]), ("SMr",), ("SMr",))
            dve(lambda e: e.tensor_scalar(dg, ident_f, SM[:, 7:8], None, op0=ALU.mult), ("SMr", "cstf"), (kdg,))

        def emit_PV(t):
            h, i, nk, nchk, own, gated = geom(t)
            hb = h % 2
            b2 = t % 2
            qs = slice(i * 128, (i + 1) * 128)
            pb, pt, dg, vh = PB[b2], PT[b2], DG[b2], VH[hb]
            kpb, kpt, kdg = ("PB", b2), ("PT", b2), ("DG", b2)
            ngrp = (i + 1 + 3) // 4
            for gI in range(ngrp):
                bk = 4 + (gI % 2)
                nb_ = min(4, i + 1 - gI * 4)

                def t_fn(e, gI=gI, nb_=nb_, bk=bk):
                    ins = None
                    for j in range(nb_):
                        kb = gI * 4 + j
                        ins = e.matmul(ps_all[:, bk * 512 + j * 128: bk * 512 + (j + 1) * 128],
                                       lhsT=pb[:, kb * 128:(kb + 1) * 128], rhs=dg, start=True, stop=True)
                    return ins
                P.op("pe", t_fn, (kpb, kdg), (("ps", bk),))
                act_copy(pt[:, gI * 4:gI * 4 + nb_, :],
                         ps_all[:, bk * 512: bk * 512 + nb_ * 128].rearrange("p (j q) -> p j q", j=nb_),
                         (("ps", bk),), (kpt,))
            pe_acc(ps_all[:, 6 * 512:6 * 512 + 128], [(vh[:, kb, :], pt[:, kb, :]) for kb in range(i + 1)],
                   (("VH", hb), kpt), (("ps", 6),))
            act_copy(R1[:, h, qs], ps_all[:, 6 * 512:6 * 512 + 128], (("ps", 6),), (("R1o", h),))

        ntile = nheads * NT
        loads(0)
        emit_S(0)
        ada_jt = 0
        for t in range(ntile):
            if t % NT == 0 and t // NT + 1 < nheads:
                loads(t // NT + 1)
            if ada_l is not None and t % 10 == 5 and ada_jt < 24:
                ada_tile(ada_l, ada_jt)
                ada_jt += 1
            emit_softmax(t)
            if t + 1 < ntile:
                emit_S(t + 1)
            emit_PV(t)
        if ada_l is not None:
            while ada_jt < 24:
                ada_tile(ada_l, ada_jt)
                ada_jt += 1
            ada_finish(ada_l)

    def out_proj(w):
        YST = [carve_t([TC], F32) for _ in range(3)]
        src = w.rearrange("(k p) n -> p k n", p=128)
        cnt = 0
        for ct in range(4):
            ri, rk = ring_next()
            wt = ring[ri][:, 0:16 * 512].rearrange("p (k n) -> p k n", k=16)
            wdma(wt, src[:, :, ct * 512:(ct + 1) * 512], rk)
            for mb in range(4):
                m = ct * 4 + mb
                for n in range(NCH):
                    cs = slice(n * TC, (n + 1) * TC)
                    bk = cnt % 8
                    b3 = cnt % 3
                    cnt += 1
                    pe_acc(bank(bk), [(wt[:, k, mb * 128:(mb + 1) * 128], R1[:, k, cs]) for k in range(16)],
                           (rk,) + tuple(("R1o", k) for k in range(16)), (("ps", bk),))
                    act_copy(YST[b3], bank(bk), (("ps", bk),), (("YST", b3),))
                    dma_sp(yS[m, :, cs], YST[b3], (("YST", b3),), (("yS", n, m),), stream="sp_st")

    def mla_mixer(j):
        phase_reset()
        nonlocal KR
        KR = carve_t([T], BF16)
        CN = carve_t([8 * T], BF16).rearrange("p (k n) -> p k n", k=8)
        CB = carve_t([4 * TC], F32).rearrange("p (k n) -> p k n", k=4)
        SQ = [carve_t([TC], F32) for _ in range(2)]
        RS = carve_t([TC], F32)
        TB = [carve_t([TC], F32) for _ in range(2)]
        T1 = carve_t([TC], F32)
        T2 = carve_t([TC], F32)
        wsrc = mla_w_down[j].rearrange("(k p) n -> p k n", p=128)
        r1keys = tuple(("R1", n) for n in range(NCH))

        def rms_group(wt, rk, cb0, goff, n, cs):
            for m in range(4):
                bk = m
                pe_acc(bank(bk), [(wt[:, k, (cb0 + m) * 128:(cb0 + m + 1) * 128], R1[:, k, cs]) for k in range(16)],
                       (rk, ("R1", n)), (("ps", bk),))
                act_copy(CB[:, m, :], bank(bk), (("ps", bk),), (("CB", m),))
                sq = SQ[m % 2]
                act(lambda e, sq=sq, bk=bk: e.activation(sq, bank(bk), AF.Square), (("ps", bk),), (("SQ", m % 2),))
                P.op("pe", lambda e, sq=sq, m=m: e.matmul(bank(4), lhsT=ones_f, rhs=sq, start=(m == 0), stop=(m == 3)),
                     (("SQ", m % 2), "cstf"), (("ps", 4),))
            dve(lambda e: e.tensor_scalar(RS, bank(4), 1.0 / 512.0, RMS_EPS, op0=ALU.mult, op1=ALU.add),
                (("ps", 4),), ("RS",))
            act(lambda e: e.activation(RS, RS, AF.Sqrt), ("RS",), ("RS",))
            dve(lambda e: e.reciprocal(RS, RS), ("RS",), ("RS",))
            for m in range(4):
                gcol = qkvn[:, j * 8 + goff + m: j * 8 + goff + m + 1]
                dve(lambda e, m=m, gcol=gcol: e.scalar_tensor_tensor(CN[:, goff + m, cs], CB[:, m, :], gcol, RS,
                                                                     op0=ALU.mult, op1=ALU.mult),
                    (("CB", m), "RS", "vecs"), (("CN", goff + m),))

        ri, rk = ring_next()
        wt = ring[ri][:, 0:16 * 512].rearrange("p (k n) -> p k n", k=16)
        wdma(wt, wsrc[:, :, 0:512], rk)
        for n in range(NCH):
            rms_group(wt, rk, 0, 0, n, slice(n * TC, (n + 1) * TC))
        ri, rk = ring_next()
        wt = ring[ri][:, 0:16 * 640].rearrange("p (k n) -> p k n", k=16)
        wdma(wt[:, :, 0:576], wsrc[:, :, 512:1088], rk)
        dve(lambda e, wt=wt: e.tensor_copy(wt[:, :, 576:608], wt[:, :, 544:576]), (rk,), (rk,))
        dve(lambda e, wt=wt: e.tensor_copy(wt[:, :, 608:640], wt[:, :, 512:544]), (rk,), (rk,))
        for n in range(NCH):
            cs = slice(n * TC, (n + 1) * TC)
            rms_group(wt, rk, 0, 4, n, cs)
            dma_sp(TB[0][0:64, :], tab[0, :, cs], (("tab", 0),), (("TB", 0),))
            dma_sp(TB[1][0:64, :], tab[1, :, cs], (("tab", 1),), (("TB", 1),))
            pe_acc(ps_all[0:64, 5 * 512:6 * 512], [(wt[:, k, 512:576], R1[:, k, cs]) for k in range(16)],
                   (rk, ("R1", n)), (("ps", 5),))
            pe_acc(ps_all[0:64, 6 * 512:7 * 512], [(wt[:, k, 576:640], R1[:, k, cs]) for k in range(16)],
                   (rk, ("R1", n)), (("ps", 6),))
            dve(lambda e: e.tensor_tensor(T1[0:64, :], ps_all[0:64, 5 * 512:6 * 512], TB[0][0:64, :], op=ALU.mult),
                (("ps", 5), ("TB", 0)), ("T1",))
            dve(lambda e: e.tensor_tensor(T2[0:64, :], ps_all[0:64, 6 * 512:7 * 512], TB[1][0:64, :], op=ALU.mult),
                (("ps", 6), ("TB", 1)), ("T2",))
            dve(lambda e, cs=cs: e.tensor_tensor(KR[0:64, cs], T1[0:64, :], T2[0:64, :], op=ALU.add),
                ("T1", "T2"), ("KR",))
        QST = [carve_t([T], BF16) for _ in range(2)]
        QRS = [carve_t([T], BF16) for _ in range(2)]
        usrc = mla_w_uq[j].rearrange("(k p) n -> p k n", p=128)
        for hg in range(2):
            ri, rk = ring_next()
            wt = ring[ri][:, 0:4 * 2048].rearrange("p (k n) -> p k n", k=4)
            wdma(wt[:, :, 0:1536], usrc[:, :, hg * 1536:(hg + 1) * 1536], rk)
            for hh in range(8):
                dve(lambda e, wt=wt, hh=hh: e.tensor_copy(wt[:, :, 1536 + hh * 64:1536 + hh * 64 + 32],
                                                          wt[:, :, hh * 192 + 160:hh * 192 + 192]), (rk,), (rk,))
                dve(lambda e, wt=wt, hh=hh: e.tensor_copy(wt[:, :, 1536 + hh * 64 + 32:1536 + hh * 64 + 64],
                                                          wt[:, :, hh * 192 + 128:hh * 192 + 160]), (rk,), (rk,))
            for hh in range(8):
                h = hg * 8 + hh
                hb = h % 2
                for n in range(NCH):
                    cs = slice(n * TC, (n + 1) * TC)
                    cnr = tuple(("CN", k) for k in range(4))
                    pe_acc(bank(0), [(wt[:, k, hh * 192:hh * 192 + 128], CN[:, k, cs]) for k in range(4)],
                           (rk,) + cnr, (("ps", 0),))
                    pe_acc(ps_all[0:64, 512:1024], [(wt[:, k, hh * 192 + 128:hh * 192 + 192], CN[:, k, cs]) for k in range(4)],
                           (rk,) + cnr, (("ps", 1),))
                    pe_acc(ps_all[0:64, 1024:1536], [(wt[:, k, 1536 + hh * 64:1536 + hh * 64 + 64], CN[:, k, cs]) for k in range(4)],
                           (rk,) + cnr, (("ps", 2),))
                    act_copy(QST[hb][:, cs], bank(0), (("ps", 0),), (("QST", hb),))
                    dma_sp(TB[0][0:64, :], tab[0, :, cs], (("tab", 0),), (("TB", 0),))
                    dma_sp(TB[1][0:64, :], tab[1, :, cs], (("tab", 1),), (("TB", 1),))
                    dve(lambda e: e.tensor_tensor(T1[0:64, :], ps_all[0:64, 512:1024], TB[0][0:64, :], op=ALU.mult),
                        (("ps", 1), ("TB", 0)), ("T1",))
                    dve(lambda e: e.tensor_tensor(T2[0:64, :], ps_all[0:64, 1024:1536], TB[1][0:64, :], op=ALU.mult),
                        (("ps", 2), ("TB", 1)), ("T2",))
                    dve(lambda e, cs=cs, hb=hb: e.tensor_tensor(QRS[hb][0:64, cs], T1[0:64, :], T2[0:64, :], op=ALU.add),
                        ("T1", "T2"), (("QRS", hb),))
                dma_sp(qn_d[h, :, :], QST[hb], (("QST", hb),), (("qn_d", h),), stream="sp_st")
                dma_sp(qr_d[h, :, :], QRS[hb][0:64, :], (("QRS", hb),), (("qr_d", h),), stream="sp_st")
        VST = [carve_t([TC], BF16) for _ in range(3)]
        ksrc = mla_w_ukv[j].rearrange("(k p) n -> p k n", p=128)
        cnt = 0
        for hg in range(2):
            ri, rk = ring_next()
            wt = ring[ri][:, 0:4 * 2048].rearrange("p (k n) -> p k n", k=4)
            wdma(wt, ksrc[:, :, hg * 2048:(hg + 1) * 2048], rk)
            cnr = tuple(("CN", 4 + k) for k in range(4))
            for hh in range(8):
                h = hg * 8 + hh
                hb = h % 2
                for n in range(NCH):
                    cs = slice(n * TC, (n + 1) * TC)
                    bk = cnt % 4
                    cnt += 1
                    pe_acc(bank(bk), [(wt[:, k, hh * 256:hh * 256 + 128], CN[:, 4 + k, cs]) for k in range(4)],
                           (rk,) + cnr, (("ps", bk),))
                    act_copy(QST[hb][:, cs], bank(bk), (("ps", bk),), (("QST", hb),))
                dma_sp(kn_d[h, :, :], QST[hb], (("QST", hb),), (("kn_d", h),), stream="sp_st")
            for tt in range(NT):
                ts_ = slice(tt * 128, (tt + 1) * 128)
                for g2 in range(2):
                    bk = 4 + cnt % 4
                    b3 = cnt % 3
                    cnt += 1
                    for hs in range(4):
                        hc = (g2 * 4 + hs) * 256 + 128
                        pe_acc(ps_all[:, bk * 512 + hs * 128: bk * 512 + (hs + 1) * 128],
                               [(CN[:, 4 + k, ts_], wt[:, k, hc:hc + 128]) for k in range(4)],
                               (rk,) + cnr, (("ps", bk),))
                    act_copy(VST[b3], bank(bk), (("ps", bk),), (("VST", b3),))
                    c0 = (hg * 8 + g2 * 4) * 128
                    dma_sp(v_d[ts_, c0:c0 + 512], VST[b3], (("VST", b3),), (("v_d", tt, c0 // 512),), stream="sp_st")
        P.barrier()
        aoff[0] = A_BASE + ((T * 2 + 63) // 64) * 64
        attention("mla", ada_l=(2 * j + 1 if 2 * j + 1 < nlayers else None))
        if j == 0:
            dump_r1("oT0", tuple(("R1o", h) for h in range(16)))
        P.barrier()
        phase_reset()
        out_proj(mla_w_o[j])
        P.barrier()

    KR = None

    def moba_mixer(j):
        phase_reset()
        if int(os.environ.get("K_STOP", "9")) <= 0:
            return
        QST = [carve_t([T], BF16) for _ in range(2)]
        VST = [carve_t([TC], BF16) for _ in range(3)]
        TB = [carve_t([TC], F32) for _ in range(2)]
        T1 = carve_t([TC], F32)
        T2 = carve_t([TC], F32)
        RO = carve_t([TC], BF16)
        wsrc = moba_w_qkv[j].rearrange("(k p) n -> p k n", p=128)
        r1keys = tuple(("R1", n) for n in range(NCH))
        cnt = 0
        for qk in range(2):
            dst = qn_d if qk == 0 else kn_d
            dkey = "qn_d" if qk == 0 else "kn_d"
            for hg in range(4):
                ri, rk = ring_next()
                wt = ring[ri][:, 0:16 * 768].rearrange("p (k n) -> p k n", k=16)
                c0 = qk * 2048 + hg * 512
                wdma(wt[:, :, 0:512], wsrc[:, :, c0:c0 + 512], rk)
                for hh in range(4):
                    wdma(wt[:, :, 512 + hh * 32:512 + hh * 32 + 16], wsrc[:, :, c0 + hh * 128 + 16:c0 + hh * 128 + 32], rk)
                    wdma(wt[:, :, 512 + hh * 32 + 16:512 + hh * 32 + 32], wsrc[:, :, c0 + hh * 128:c0 + hh * 128 + 16], rk)
                wdma(wt[:, :, 640:768], wsrc[:, :, c0:c0 + 128], rk)
                for hh in range(4):
                    h = hg * 4 + hh
                    hb = h % 2
                    for n in range(NCH):
                        cs = slice(n * TC, (n + 1) * TC)
                        bk = cnt % 2
                        cnt += 1
                        pe_acc(bank(bk), [(wt[:, k, hh * 128:(hh + 1) * 128], R1[:, k, cs]) for k in range(16)],
                               (rk, ("R1", n)), (("ps", bk),))
                        pe_acc(ps_all[:, (2 + bk) * 512:(3 + bk) * 512],
                               [(wt[:, k, 512 + hh * 32:512 + hh * 32 + 128], R1[:, k, cs]) for k in range(16)],
                               (rk, ("R1", n)), (("ps", 2 + bk),))
                        act_copy(QST[hb][:, cs], bank(bk), (("ps", bk),), (("QST", hb),))
                        if os.environ.get("K_NOROPE"):
                            continue
                        dma_sp(TB[0][0:32, :], tab[2, 0:32, cs], (("tab", 2),), (("TB", 0),))
                        dma_sp(TB[1][0:32, :], tab[3, 0:32, cs], (("tab", 3),), (("TB", 1),))
                        dve(lambda e, bk=bk: e.tensor_tensor(T1[0:32, :], ps_all[0:32, bk * 512:(bk + 1) * 512],
                                                             TB[0][0:32, :], op=ALU.mult), (("ps", bk), ("TB", 0), ("QST", hb)), ("T1",))
                        dve(lambda e, bk=bk: e.tensor_tensor(T2[0:32, :], ps_all[0:32, (2 + bk) * 512:(3 + bk) * 512],
                                                             TB[1][0:32, :], op=ALU.mult), (("ps", 2 + bk), ("TB", 1)), ("T2",))
                        dve(lambda e, cs=cs, hb=hb: e.tensor_tensor(QST[hb][0:32, cs], T1[0:32, :], T2[0:32, :], op=ALU.add),
                            ("T1", "T2", ("QST", hb)), (("QST", hb),))
                    dma_sp(dst[h, :, :], QST[hb], (("QST", hb),), ((dkey, h),), stream="sp_st")
        KS = int(os.environ.get("K_STOP", "9"))
        if KS <= 1:
            P.barrier()
            return
        for hg in range(4):
            ri, rk = ring_next()
            wt = ring[ri][:, 0:16 * 512].rearrange("p (k n) -> p k n", k=16)
            c0 = 4096 + hg * 512
            wdma(wt, wsrc[:, :, c0:c0 + 512], rk)
            for tt in range(NT):
                ts_ = slice(tt * 128, (tt + 1) * 128)
                bk = 4 + cnt % 4
                b3 = cnt % 3
                cnt += 1
                pe_acc(bank(bk), [(R1[:, k, ts_], wt[:, k, :]) for k in range(16)], (rk,) + r1keys, (("ps", bk),))
                act_copy(VST[b3], bank(bk), (("ps", bk),), (("VST", b3),))
                dma_sp(v_d[ts_, hg * 512:(hg + 1) * 512], VST[b3], (("VST", b3),), (("v_d", tt, hg),), stream="sp_st")
        P.barrier()
        if KS <= 2:
            return
        phase_reset()
        attention("moba", ada_l=(2 * j + 2 if 2 * j + 2 < nlayers else None))
        P.barrier()
        if KS <= 3:
            return
        phase_reset()
        out_proj(moba_w_o[j])
        P.barrier()

    def swiglu_pass(xs_fn, xs_keys, N, wgu, wdn, mode, H, out_fn):
        SG = [carve_t([CAP], F32) for _ in range(2)]
        gsrc = wgu.rearrange("(k p) n -> p k n", p=128)
        dsrc = wdn.rearrange("(k p) n -> p k n", p=128)
        chunks = [(0, min(512, N))] + ([(512, N - 512)] if N > 512 else [])
        cnt = 0
        for tg in range(22):
            ri, rk = ring_next()
            wt = ring[ri][:, 0:16 * 512].rearrange("p (k n) -> p k n", k=16)
            wdma(wt[:, :, 0:256], gsrc[:, :, tg * 256:(tg + 1) * 256], rk)
            wdma(wt[:, :, 256:512], gsrc[:, :, DFF + tg * 256:DFF + (tg + 1) * 256], rk)
            for b2 in range(2):
                jb = tg * 2 + b2
                a2 = cnt % 2
                cnt += 1
                gb, ub = 4 * a2, 4 * a2 + 2
                for (base, col0) in ((gb, b2 * 128), (ub, 256 + b2 * 128)):
                    def fn(e, base=base, col0=col0, wt=wt):
                        ins = None
                        for ci, (c0, w) in enumerate(chunks):
                            o = (base + ci) * 512
                            for k in range(16):
                                ins = e.matmul(ps_all[:, o:o + w], lhsT=wt[:, k, col0:col0 + 128],
                                               rhs=xs_fn(k)[:, c0:c0 + w], start=(k == 0), stop=(k == 15))
                        return ins
                    P.op("pe", fn, (rk,) + tuple(xs_keys), (("ps", base), ("ps", base + 1)))
                sg = SG[a2]
                for ci, (c0, w) in enumerate(chunks):
                    act(lambda e, sg=sg, gb=gb, ci=ci, c0=c0, w=w: e.activation(
                        sg[:, c0:c0 + w], ps_all[:, (gb + ci) * 512:(gb + ci) * 512 + w], AF.Silu),
                        (("ps", gb), ("ps", gb + 1)), (("SG", a2),))
                    dve(lambda e, sg=sg, ub=ub, ci=ci, c0=c0, w=w, jb=jb: e.tensor_tensor(
                        H[:, jb, c0:c0 + w], sg[:, c0:c0 + w], ps_all[:, (ub + ci) * 512:(ub + ci) * 512 + w], op=ALU.mult),
                        (("SG", a2), ("ps", ub), ("ps", ub + 1)), (("H", jb),))
        hkeys = tuple(("H", jb) for jb in range(NKF))
        cnt = 0
        for tw in range(8):
            ri, rk = ring_next()
            wt = ring[ri][:, 0:NKF * 256].rearrange("p (k n) -> p k n", k=NKF)
            wdma(wt[:, 0:22, :], dsrc[:, 0:22, tw * 256:(tw + 1) * 256], rk)
            wdma(wt[:, 22:44, :], dsrc[:, 22:44, tw * 256:(tw + 1) * 256], rk)
            if mode == "B":
                for b2 in range(2):
                    m = tw * 2 + b2
                    a2 = cnt % 4
                    cnt += 1
                    base = 2 * a2

                    def fn(e, base=base, b2=b2, wt=wt):
                        ins = None
                        for ci, (c0, w) in enumerate(chunks):
                            o = (base + ci) * 512
                            for k in range(NKF):
                                ins = e.matmul(ps_all[:, o:o + w], lhsT=wt[:, k, b2 * 128:(b2 + 1) * 128],
                                               rhs=H[:, k, c0:c0 + w], start=(k == 0), stop=(k == NKF - 1))
                        return ins
                    P.op("pe", fn, (rk,) + hkeys, (("ps", base), ("ps", base + 1)))
                    out_fn(m, [(ps_all[:, (base + ci) * 512:(base + ci) * 512 + w], c0, w, base + ci)
                               for ci, (c0, w) in enumerate(chunks)])
            else:
                for st in range(NST):
                    bk = cnt % 8
                    cnt += 1
                    pe_acc(ps_all[:, bk * 512:bk * 512 + 256],
                           [(H[:, k, st * 128:(st + 1) * 128], wt[:, k, :]) for k in range(NKF)],
                           (rk,) + hkeys, (("ps", bk),))
                    out_fn(tw, st, ps_all[:, bk * 512:bk * 512 + 256], bk)

    def dense_ffn(j):
        phase_reset()
        H = carve_t([NKF * CAP], BF16).rearrange("p (k n) -> p k n", k=NKF)
        YST = [carve_t([TC], F32) for _ in range(3)]
        ycnt = [0]
        for n in range(NCH):
            cs = slice(n * TC, (n + 1) * TC)

            def out_fn(m, lst, cs=cs, n=n):
                for (ap, c0, w, bk) in lst:
                    b3 = ycnt[0] % 3
                    ycnt[0] += 1
                    act_copy(YST[b3], ap, (("ps", bk),), (("YST", b3),))
                    dma_sp(yS[m, :, cs], YST[b3], (("YST", b3),), (("yS", n, m),), stream="sp_st")
            swiglu_pass(lambda k, cs=cs: R1[:, k, cs], (("R1", n),), TC, ffn_w_gate_up[j], ffn_w_down[j], "B", H, out_fn)
            aoff[0] -= 2 * ((CAP * 4 + 63) // 64 * 64)
        P.barrier()

    def moe_ffn(j):
        phase_reset()
        GW = RT[:, 0:128].rearrange("p (t e) -> p t e", e=8)
        SEL = RT[:, 128:256].rearrange("p (t e) -> p t e", e=8)
        POS = RT[:, 256:384].rearrange("p (t e) -> p t e", e=8)
        LG = RT[:, 384:392]
        M8 = RT[:, 392:400]
        EX = RT[:, 400:408]
        NG = RT[:, 408:409]
        DEN = RT[:, 409:410]
        SELB = RTB[:, :].rearrange("p (t e) -> p t e", e=8)
        WRv = WR[:, :].rearrange("p (k e) -> p k e", e=8)
        r1keys = tuple(("R1", n) for n in range(NCH))
        P.op("pool", lambda e: e.dma_start(out=WRv, in_=moe_w_router[j].rearrange("(k p) e -> p k e", p=128)),
             (), ("WR",), dma="pool_WR")
        for tt in range(NT):
            ts_ = slice(tt * 128, (tt + 1) * 128)
            bk = tt % 2
            pe_acc(ps_all[:, bk * 512:bk * 512 + 8], [(R1[:, k, ts_], WRv[:, k, :]) for k in range(16)],
                   ("WR",) + r1keys, (("ps", bk),))
            dve(lambda e, bk=bk: e.tensor_copy(LG, ps_all[:, bk * 512:bk * 512 + 8]), (("ps", bk),), ("LG",))
            dve(lambda e: e.max(M8, LG), ("LG",), ("M8",))
            dve(lambda e, tt=tt: e.tensor_scalar(SEL[:, tt, :], LG, M8[:, 1:2], None, op0=ALU.is_ge), ("LG", "M8"), ("SEL",))
            dve(lambda e: e.tensor_scalar(NG, M8[:, 0:1], -1.0, None, op0=ALU.mult), ("M8",), ("NG",))
            act(lambda e: e.activation(EX, LG, AF.Exp, bias=NG, scale=1.0), ("LG", "NG"), ("EX",))
            dve(lambda e, tt=tt: e.tensor_tensor(EX, EX, SEL[:, tt, :], op=ALU.mult), ("EX", "SEL"), ("EX",))
            dve(lambda e: e.reduce_sum(DEN, EX, axis=AX.X), ("EX",), ("DEN",))
            dve(lambda e: e.reciprocal(DEN, DEN), ("DEN",), ("DEN",))
            dve(lambda e, tt=tt: e.tensor_scalar(GW[:, tt, :], EX, DEN, None, op0=ALU.mult), ("EX", "DEN"), ("GW",))
            dve(lambda e, tt=tt: e.tensor_copy(SELB[:, tt, :], SEL[:, tt, :]), ("SEL",), ("SELB",))
        for tt in range(NT):
            bk = 2 + tt % 2
            pairs = [(ones_b, SELB[:, t2, :]) for t2 in range(tt)] + [(ltri, SELB[:, tt, :])]
            pe_acc(ps_all[:, bk * 512:bk * 512 + 8], pairs, ("SELB", "cstb"), (("ps", bk),))
            dve(lambda e, tt=tt, bk=bk: e.tensor_copy(POS[:, tt, :], ps_all[:, bk * 512:bk * 512 + 8]),
                (("ps", bk),), ("POS",))
        UST = [carve_t([2048], BF16) for _ in range(2)]
        for tt in range(NT):
            ts_ = slice(tt * 128, (tt + 1) * 128)
            ub = tt % 2
            for g in range(4):
                bk = 4 + (tt * 4 + g) % 4

                def fn(e, g=g, bk=bk, ts_=ts_):
                    ins = None
                    for q in range(4):
                        fb = g * 4 + q
                        ins = e.transpose(ps_bf[:, bk * 1024 + q * 128: bk * 1024 + (q + 1) * 128], R1[:, fb, ts_], ident_b)
                    return ins
                P.op("pe", fn, r1keys + ("cstb",), (("ps", bk),))
                act_copy(UST[ub][:, g * 512:(g + 1) * 512], ps_bf[:, bk * 1024: bk * 1024 + 512], (("ps", bk),), (("UST", ub),))
            dma_sp(ut_d[ts_, :], UST[ub], (("UST", ub),), (("ut_d", tt),), stream="sp_st")
        P.barrier()
        aoff[0] = 0
        H = carve_t([NKF * CAP], BF16).rearrange("p (k n) -> p k n", k=NKF)
        assert aoff[0] >= A_BASE
        XS = carve_t([NFB * CAP], BF16).rearrange("p (k n) -> p k n", k=NFB)
        PEB = carve_t([NT * CAP], BF16).rearrange("p (t n) -> p t n", t=NT)
        PW = [carve_t([CAP], BF16) for _ in range(2)]
        PTS = [carve_t([NST * 128], BF16).rearrange("p (s q) -> p s q", s=NST) for _ in range(2)]
        UTB = [carve_t([NT * 128], BF16).rearrange("p (t f) -> p t f", t=NT) for _ in range(3)]
        YSS = [carve_t([256], BF16) for _ in range(4)]
        cnt_t = 0
        cnt_u = 0
        ycnt = [0]
        for ex in range(NE):
            for tt in range(NT):
                ts_ = slice(tt * 128, (tt + 1) * 128)
                dve(lambda e, tt=tt, ex=ex: e.tensor_scalar(PEB[:, tt, :], iota_s, POS[:, tt, ex:ex + 1], SEL[:, tt, ex:ex + 1],
                                                            op0=ALU.is_equal, op1=ALU.mult),
                    ("POS", "SEL", "cstf"), (("PEB", tt),))
                pw = PW[tt % 2]
                dve(lambda e, tt=tt, ex=ex, pw=pw: e.tensor_scalar(pw, PEB[:, tt, :], GW[:, tt, ex:ex + 1], None, op0=ALU.mult),
                    (("PEB", tt), "GW"), (("PW", tt % 2),))
                bk = 6 + cnt_t % 2
                pts = PTS[cnt_t % 2]
                kpts = ("PTS", cnt_t % 2)
                cnt_t += 1

                def fn(e, pw=pw, bk=bk):
                    ins = None
                    for st in range(NST):
                        ins = e.transpose(ps_bf[:, bk * 1024 + st * 128: bk * 1024 + (st + 1) * 128],
                                          pw[:, st * 128:(st + 1) * 128], ident_b)
                    return ins
                P.op("pe", fn, (("PW", tt % 2), "cstb"), (("ps", bk),))
                act_copy(pts, ps_bf[:, bk * 1024: bk * 1024 + NST * 128].rearrange("p (s q) -> p s q", s=NST),
                         (("ps", bk),), (kpts,))
                dma_sp(ptw_d[ex, :, :, ts_].rearrange("s p q -> p s q"), pts, (kpts,), (("ptw_d", ex, tt),), stream="sp_st")
            pebkeys = tuple(("PEB", tt) for tt in range(NT))
            for fb in range(NFB):
                utb = UTB[cnt_u % 3]
                kut = ("UTB", cnt_u % 3)
                a2 = cnt_u % 2
                cnt_u += 1
                dma_sp(utb, ut_d[:, fb * 128:(fb + 1) * 128].rearrange("(t p) f -> p t f", p=128), tuple(("ut_d", t_) for t_ in range(NT)), (kut,))
                base = 2 * a2

                def fn(e, utb=utb, base=base):
                    ins = None
                    for ci, (c0, w) in enumerate(((0, 512), (512, CAP - 512))):
                        o = (base + ci) * 512
                        for tt in range(NT):
                            ins = e.matmul(ps_all[:, o:o + w], lhsT=utb[:, tt, :], rhs=PEB[:, tt, c0:c0 + w],
                                           start=(tt == 0), stop=(tt == NT - 1))
                    return ins
                P.op("pe", fn, (kut,) + pebkeys, (("ps", base), ("ps", base + 1)))
                act_copy(XS[:, fb, 0:512], ps_all[:, base * 512:(base + 1) * 512], (("ps", base),), (("XS", fb),))
                act_copy(XS[:, fb, 512:CAP], ps_all[:, (base + 1) * 512:(base + 1) * 512 + CAP - 512], (("ps", base + 1),), (("XS", fb),))

            def out_fn(tw, st, ap, bk, ex=ex):
                b4 = ycnt[0] % 4
                ycnt[0] += 1
                act_copy(YSS[b4], ap, (("ps", bk),), (("YSS", b4),))
                dma_sp(ys_d[ex, st * 128:(st + 1) * 128, tw * 256:(tw + 1) * 256], YSS[b4], (("YSS", b4),), (("ys_d", ex, st, tw),),
                       stream="sp_st")
            mark = aoff[0]
            swiglu_pass(lambda k: XS[:, k, :], tuple(("XS", fb) for fb in range(NFB)), CAP,
                        moe_w_gate_up[j, ex], moe_w_down[j, ex], "A", H, out_fn)
            aoff[0] = mark
        P.barrier()
        phase_reset()
        YSB = [carve_t([1024], BF16) for _ in range(4)]
        PTB = [carve_t([TC], BF16) for _ in range(4)]
        YST = [carve_t([TC], F32) for _ in range(3)]
        cnt = 0
        ycnt2 = 0
        for n in range(NCH):
            cs = slice(n * TC, (n + 1) * TC)
            for half in range(2):
                for ex in range(NE):
                    for st in range(NST):
                        b4 = cnt % 4
                        cnt += 1
                        dma_sp(YSB[b4], ys_d[ex, st * 128:(st + 1) * 128, half * 1024:(half + 1) * 1024], tuple(("ys_d", ex, st, half * 4 + t_) for t_ in range(4)), (("YSB", b4),))
                        dma_sp(PTB[b4], ptw_d[ex, st, :, cs], tuple(("ptw_d", ex, n * 4 + t_) for t_ in range(4)), (("PTB", b4),))
                        first = (ex == 0 and st == 0)
                        lastm = (ex == NE - 1 and st == NST - 1)

                        def fn(e, b4=b4, first=first, lastm=lastm):
                            ins = None
                            for f8 in range(8):
                                ins = e.matmul(bank(f8), lhsT=YSB[b4][:, f8 * 128:(f8 + 1) * 128], rhs=PTB[b4],
                                               start=first, stop=lastm)
                            return ins
                        P.op("pe", fn, (("YSB", b4), ("PTB", b4)), tuple(("ps", f8) for f8 in range(8)))
                for f8 in range(8):
                    b3 = ycnt2 % 3
                    ycnt2 += 1
                    act_copy(YST[b3], bank(f8), (("ps", f8),), (("YST", b3),))
                    dma_sp(yS[half * 8 + f8, :, cs], YST[b3], (("YST", b3),), (("yS", n, half * 8 + f8),), stream="sp_st")
        P.barrier()

    ln_phase(0, 0, True, False)
    s = 0
    for l in range(DEPTH):
        j = l // 2
        for sub in range(2):
            if s >= nsub:
                break
            if sub == 0:
                if l % 2 == 0:
                    mla_mixer(j)
                else:
                    moba_mixer(j)
            else:
                if l % 2 == 0:
                    dense_ffn(j)
                else:
                    moe_ffn(j)
            s += 1
            ln_phase(l, sub, False, s == nsub)
    P.barrier(engines=("sp",))

    with ExitStack() as es:
        P.emit(es)
    return nc


def _consts():
    cf = np.zeros((128, 1024), np.float32)
    cf[:, 0:128] = np.eye(128, dtype=np.float32)
    cf[:, 128:128 + CAP] = np.arange(CAP, dtype=np.float32)[None, :]
    om = np.zeros((8, 8), np.float32)
    for own in range(8):
        om[own, own:] = NEGB
    extra = np.zeros((128, 96), np.float32)
    extra[:, 0:64] = om.reshape(1, 64)
    p = np.arange(128)
    invm = np.zeros(128, np.float32)
    invm[:64] = (np.float32(THETA) ** (-(np.arange(0, 64, 2, dtype=np.float32)) / np.float32(64)))[p[:64] % 32]
    invb = np.zeros(128, np.float32)
    invb[:32] = (np.float32(THETA) ** (-(np.arange(0, 32, 2, dtype=np.float32)) / np.float32(32)))[p[:32] % 16]
    extra[:, 64] = invm
    extra[:, 65] = invb
    sg = np.ones(128, np.float32)
    sg[:32] = -1.0
    extra[:, 66] = sg
    sg2 = np.ones(128, np.float32)
    sg2[:16] = -1.0
    extra[:, 67] = sg2
    cf[:, 896:1024] = 1.0
    cb = np.zeros((128, 1024), np.float32)
    cb[:, 0:128] = np.eye(128)
    q = np.arange(128)[:, None]
    k = np.arange(128)[None, :]
    cb[:, 128:256] = np.where(k <= q, 0.0, NEGB)
    cb[:, 256:384] = (q < k).astype(np.float32)
    cb[:, 384:512] = 1.0
    return cf, cb.astype(ml_dtypes.bfloat16), extra


def _fm(v):
    v = np.asarray(v, np.float32)
    return np.ascontiguousarray(v.reshape(-1, 128).T)


_NC_CACHE = {}


def _prep_core(b, inputs, cf, cb, extra):
    x = np.asarray(inputs["x"][b], np.float32)
    xT = np.ascontiguousarray(x.T).reshape(NFB, 128, T)
    vecs = np.zeros((128, 512), np.float32)
    vecs[:, 0:384] = _fm(np.asarray(inputs["b_ada"], np.float32).reshape(-1))
    vecs[:, 384:400] = _fm(inputs["c"][b])
    qk = np.zeros((128, 16), np.float32)
    for j in range(2):
        qk[:, j * 8:j * 8 + 4] = _fm(inputs["mla_q_norm"][j])
        qk[:, j * 8 + 4:j * 8 + 8] = _fm(inputs["mla_kv_norm"][j])
    vecs[:, 400:416] = qk
    vecs[:, 416:512] = extra
    lnp = np.zeros((128, 256), np.float32)
    for l in range(DEPTH):
        for idx, nm in enumerate(("ln_mix_g", "ln_mix_b", "ln_ffn_g", "ln_ffn_b")):
            lnp[:, (l * 4 + idx) * 16:(l * 4 + idx + 1) * 16] = _fm(inputs[nm][l])
    return {
        "xT": xT, "pos": np.asarray(inputs["positions"][b], np.int32).reshape(1, T),
        "cstf": cf, "cstb": cb, "vecs": vecs, "lnp": lnp,
    }


W_NAMES = ("w_ada", "mla_w_down", "mla_w_uq", "mla_w_ukv", "mla_w_o", "ffn_w_gate_up", "ffn_w_down",
           "moba_w_qkv", "moba_w_o", "moe_w_router", "moe_w_gate_up", "moe_w_down")


def run(inputs, nsub=8, dbg=False, trace=False):
    key = (nsub, dbg)
    if key not in _NC_CACHE:
        _NC_CACHE[key] = build(nsub, dbg)
    nc = _NC_CACHE[key]
    cf, cb, extra = _consts()
    names = list(W_NAMES)
    if nsub <= 1:
        names = [n for n in names if not n.startswith("ffn")]
    if nsub <= 2:
        names = [n for n in names if not n.startswith("mob")]
    if nsub <= 3:
        names = [n for n in names if not n.startswith("moe")]
    ws = {n: np.ascontiguousarray(np.asarray(inputs[n], np.float32)) for n in names}
    in_maps = []
    for b in range(NCORES):
        m = _prep_core(b, inputs, cf, cb, extra)
        m.update(ws)
        in_maps.append(m)
    res = run_bass_kernel_spmd(nc, in_maps, core_ids=list(range(NCORES)), trace=trace)
    return res


def kernel(**inputs):
    res = run(inputs)
    out = np.empty((NCORES, T, D), np.float32)
    for b in range(NCORES):
        oT = np.asarray(res.results[b]["outT"]).reshape(D, T)
        out[b] = oT.T
    return out
```
